# Optimizing a Trainium2 kernel written in Bass

```python
import math
import jax, jax.numpy as jnp
from jax import lax
import numpy as np

D_MODEL = 1024
BATCH = 8
SEQ = 4096
DEPTH = 1

CHUNK = 64
Q_BLOCK = 128
MIX_WIDTH = D_MODEL
CONV_CH = MIX_WIDTH // 2
CONV_K = 31
N_HEADS = 8
V_HEAD_DIM = (MIX_WIDTH - CONV_CH) // N_HEADS
QK_NOPE_DIM = 64
QK_ROPE_DIM = 32
QK_HEAD_DIM = QK_NOPE_DIM + QK_ROPE_DIM
Q_LORA_RANK = D_MODEL // 4
KV_LORA_RANK = D_MODEL // 8
ROPE_THETA = 10000.0
IN_WIDTH = 2 * CONV_CH + Q_LORA_RANK + KV_LORA_RANK + QK_ROPE_DIM
N_GROUPS = 4
EXPERTS_PER_GROUP = 8
N_EXPERTS = N_GROUPS * EXPERTS_PER_GROUP
TOP_K = 2
D_EXPERT = D_MODEL // 4
EPS = 1e-6

kernel_name = "hybrid_conv_mla_hmoe_block"


def rms_norm(x, g):
    xf = x.astype(jnp.float32)
    y = xf * lax.rsqrt(jnp.mean(xf * xf, axis=-1, keepdims=True) + EPS)
    return (y * g.astype(jnp.float32)).astype(x.dtype)


def layer_norm(x, g, b):
    xf = x.astype(jnp.float32)
    mu = jnp.mean(xf, axis=-1, keepdims=True)
    var = jnp.mean(jnp.square(xf - mu), axis=-1, keepdims=True)
    y = (xf - mu) * lax.rsqrt(var + EPS)
    return (y * g.astype(jnp.float32) + b.astype(jnp.float32)).astype(x.dtype)


def apply_rope(x, cos, sin):
    x1, x2 = jnp.split(x, 2, axis=-1)
    cos = cos.astype(x.dtype)
    sin = sin.astype(x.dtype)
    return jnp.concatenate([x1 * cos - x2 * sin, x1 * sin + x2 * cos], axis=-1)


def conv_group(val, gate, conv_w, conv_b, ln_g, ln_b):
    u = val * jax.nn.sigmoid(gate)
    kern = conv_w[:, None, :].astype(u.dtype)
    dw = lax.conv_general_dilated(
        u, kern, window_strides=(1,), padding=[(CONV_K - 1, 0)],
        dimension_numbers=("NWC", "WIO", "NWC"),
        feature_group_count=CONV_CH) + conv_b.astype(u.dtype)
    return jax.nn.silu(layer_norm(dw, ln_g, ln_b))


def mla_group(c_q, c_kv, k_rope, cos, sin, q_a_norm, w_q_b, kv_a_norm, w_kv_b,
              q_norm, k_norm):
    b, s, _ = c_q.shape
    q = (rms_norm(c_q, q_a_norm) @ w_q_b).reshape(b, s, N_HEADS, QK_HEAD_DIM)
    kv = (rms_norm(c_kv, kv_a_norm) @ w_kv_b).reshape(b, s, N_HEADS, QK_NOPE_DIM + V_HEAD_DIM)
    k_nope, v = kv[..., :QK_NOPE_DIM], kv[..., QK_NOPE_DIM:]
    k_r = jnp.broadcast_to(k_rope[:, :, None, :], (b, s, N_HEADS, QK_ROPE_DIM))
    k = jnp.concatenate([k_nope, k_r], axis=-1)
    q = rms_norm(q, q_norm)
    k = rms_norm(k, k_norm)
    cos_h, sin_h = cos[:, :, None, :], sin[:, :, None, :]
    q = jnp.concatenate([q[..., :QK_NOPE_DIM], apply_rope(q[..., QK_NOPE_DIM:], cos_h, sin_h)], axis=-1)
    k = jnp.concatenate([k[..., :QK_NOPE_DIM], apply_rope(k[..., QK_NOPE_DIM:], cos_h, sin_h)], axis=-1)

    n_blk = s // Q_BLOCK
    key_chunk = jnp.arange(s) // CHUNK
    scale = QK_HEAD_DIM ** -0.5
    q_blocks = q.reshape(b, n_blk, Q_BLOCK, N_HEADS, QK_HEAD_DIM).swapaxes(0, 1)

    def block_attn(args):
        qb, blk = args
        sc = jnp.einsum("bqhd,bkhd->bhqk", qb, k).astype(jnp.float32) * scale
        q_chunk = (blk * Q_BLOCK + jnp.arange(Q_BLOCK)) // CHUNK
        mask = key_chunk[None, :] <= q_chunk[:, None]
        sc = jnp.where(mask[None, None], sc, -jnp.inf)
        p = jax.nn.softmax(sc, axis=-1).astype(v.dtype)
        return jnp.einsum("bhqk,bkhd->bqhd", p, v)

    out = lax.map(block_attn, (q_blocks, jnp.arange(n_blk)))
    return out.swapaxes(0, 1).reshape(b, s, N_HEADS * V_HEAD_DIM)


def hier_moe(h, w_group, b_group, w_expert, b_expert, w_gate_e, w_up_e, w_down_e):
    n = h.shape[0]
    g_logits = (h @ w_group).astype(jnp.float32) + b_group.astype(jnp.float32)
    g_prob = jax.nn.softmax(g_logits, axis=-1)
    g_top, g_idx = lax.top_k(g_prob, 1)
    e_logits = ((h @ w_expert).astype(jnp.float32) + b_expert.astype(jnp.float32)
                ).reshape(n, N_GROUPS, EXPERTS_PER_GROUP)
    sel = jnp.broadcast_to(g_idx[:, :, None], (n, 1, EXPERTS_PER_GROUP))
    e_in = jnp.take_along_axis(e_logits, sel, axis=1)[:, 0]
    e_prob = jax.nn.softmax(e_in, axis=-1)
    e_top, e_idx = lax.top_k(e_prob, TOP_K)
    w = g_top * e_top / jnp.sum(e_top, axis=-1, keepdims=True)
    glob_idx = g_idx * EXPERTS_PER_GROUP + e_idx
    gates = jnp.sum(jax.nn.one_hot(glob_idx, N_EXPERTS, dtype=jnp.float32) * w[..., None],
                    axis=1).astype(h.dtype)
    y = jnp.zeros_like(h)
    for e in range(N_EXPERTS):
        a = jax.nn.silu(h @ w_gate_e[e]) * (h @ w_up_e[e])
        y = y + gates[:, e:e + 1] * (a @ w_down_e[e])
    return y


def hybrid_layer(x, c, cos, sin, w_ada, b_ada, norm_mix, w_in, conv_w, conv_b,
                 conv_ln_g, conv_ln_b, q_a_norm, w_q_b, kv_a_norm, w_kv_b, q_norm,
                 k_norm, w_out, norm_ffn, w_group, b_group, w_expert, b_expert,
                 w_gate_e, w_up_e, w_down_e):
    b, s, d = x.shape
    mod = jax.nn.silu(c) @ w_ada + b_ada
    sh_a, sc_a, g_a, sh_f, sc_f, g_f = jnp.split(mod, 6, axis=-1)

    h = rms_norm(x, norm_mix) * (1 + sc_a[:, None]) + sh_a[:, None]
    proj = h @ w_in
    o1 = CONV_CH
    o2 = o1 + CONV_CH
    o3 = o2 + Q_LORA_RANK
    o4 = o3 + KV_LORA_RANK
    conv_val, conv_gate, c_q, c_kv, k_rope = jnp.split(proj, [o1, o2, o3, o4], axis=-1)
    y_conv = conv_group(conv_val, conv_gate, conv_w, conv_b, conv_ln_g, conv_ln_b)
    y_attn = mla_group(c_q, c_kv, k_rope, cos, sin, q_a_norm, w_q_b, kv_a_norm,
                       w_kv_b, q_norm, k_norm)
    mixed = jnp.concatenate([y_conv, y_attn], axis=-1) @ w_out
    x = x + g_a[:, None] * mixed

    h2 = rms_norm(x, norm_ffn) * (1 + sc_f[:, None]) + sh_f[:, None]
    y = hier_moe(h2.reshape(b * s, d), w_group, b_group, w_expert, b_expert,
                 w_gate_e, w_up_e, w_down_e).reshape(b, s, d)
    return x + g_f[:, None] * y


def setup_inputs(seed: int = 0) -> dict:
    key = jax.random.key(seed)
    ks = jax.random.split(key, 32)
    f32 = jnp.float32

    def nrm(k, shape, scale):
        return jax.random.normal(k, shape, f32) * scale

    def gain(k, n):
        return 1.0 + 0.01 * jax.random.normal(k, (DEPTH, n), f32)

    positions = (jax.random.randint(ks[2], (BATCH, 1), 0, 4096, dtype=jnp.int32)
                 + jnp.arange(SEQ, dtype=jnp.int32)[None, :])
    return {
        "x": nrm(ks[0], (BATCH, SEQ, D_MODEL), 1.0),
        "c": nrm(ks[1], (BATCH, D_MODEL), 1.0),
        "positions": positions,
        "w_ada": nrm(ks[3], (DEPTH, D_MODEL, 6 * D_MODEL), 0.5 * D_MODEL ** -0.5),
        "b_ada": nrm(ks[4], (DEPTH, 6 * D_MODEL), 0.01),
        "norm_mix": gain(ks[5], D_MODEL),
        "w_in": nrm(ks[6], (DEPTH, D_MODEL, IN_WIDTH), D_MODEL ** -0.5),
        "conv_w": nrm(ks[7], (DEPTH, CONV_K, CONV_CH), CONV_K ** -0.5),
        "conv_b": nrm(ks[8], (DEPTH, CONV_CH), 0.01),
        "conv_ln_g": gain(ks[9], CONV_CH),
        "conv_ln_b": nrm(ks[10], (DEPTH, CONV_CH), 0.01),
        "q_a_norm": gain(ks[11], Q_LORA_RANK),
        "w_q_b": nrm(ks[12], (DEPTH, Q_LORA_RANK, N_HEADS * QK_HEAD_DIM), Q_LORA_RANK ** -0.5),
        "kv_a_norm": gain(ks[13], KV_LORA_RANK),
        "w_kv_b": nrm(ks[14], (DEPTH, KV_LORA_RANK, N_HEADS * (QK_NOPE_DIM + V_HEAD_DIM)), KV_LORA_RANK ** -0.5),
        "q_norm": gain(ks[15], QK_HEAD_DIM),
        "k_norm": gain(ks[16], QK_HEAD_DIM),
        "w_out": nrm(ks[17], (DEPTH, MIX_WIDTH, D_MODEL), MIX_WIDTH ** -0.5),
        "norm_ffn": gain(ks[18], D_MODEL),
        "w_group": nrm(ks[19], (DEPTH, D_MODEL, N_GROUPS), D_MODEL ** -0.5),
        "b_group": nrm(ks[20], (DEPTH, N_GROUPS), 0.01),
        "w_expert": nrm(ks[21], (DEPTH, D_MODEL, N_EXPERTS), D_MODEL ** -0.5),
        "b_expert": nrm(ks[22], (DEPTH, N_EXPERTS), 0.01),
        "w_gate_e": nrm(ks[23], (DEPTH, N_EXPERTS, D_MODEL, D_EXPERT), D_MODEL ** -0.5),
        "w_up_e": nrm(ks[24], (DEPTH, N_EXPERTS, D_MODEL, D_EXPERT), D_MODEL ** -0.5),
        "w_down_e": nrm(ks[25], (DEPTH, N_EXPERTS, D_EXPERT, D_MODEL), D_EXPERT ** -0.5),
    }


def reference(x, c, positions, w_ada, b_ada, norm_mix, w_in, conv_w, conv_b,
              conv_ln_g, conv_ln_b, q_a_norm, w_q_b, kv_a_norm, w_kv_b, q_norm,
              k_norm, w_out, norm_ffn, w_group, b_group, w_expert, b_expert,
              w_gate_e, w_up_e, w_down_e):
    inv_freq = ROPE_THETA ** (-jnp.arange(0, QK_ROPE_DIM, 2, dtype=jnp.float32) / QK_ROPE_DIM)
    ang = positions.astype(jnp.float32)[..., None] * inv_freq
    cos, sin = jnp.cos(ang), jnp.sin(ang)
    for l in range(DEPTH):
        x = hybrid_layer(x, c, cos, sin, w_ada[l], b_ada[l], norm_mix[l], w_in[l],
                         conv_w[l], conv_b[l], conv_ln_g[l], conv_ln_b[l],
                         q_a_norm[l], w_q_b[l], kv_a_norm[l], w_kv_b[l], q_norm[l],
                         k_norm[l], w_out[l], norm_ffn[l], w_group[l], b_group[l],
                         w_expert[l], b_expert[l], w_gate_e[l], w_up_e[l], w_down_e[l])
    return x
```

```python
import math
from contextlib import ExitStack

import numpy as np
import concourse.bass as bass
import concourse.mybir as mybir
from concourse.bass_utils import run_bass_kernel_spmd

F32 = mybir.dt.float32
BF16 = mybir.dt.bfloat16
I32 = mybir.dt.int32
ALU = mybir.AluOpType
AF = mybir.ActivationFunctionType
AX = mybir.AxisListType

S = 4096
D = 1024
NT = 32
NB = 8
EPS = 1e-6
import os
STAGE = int(os.environ.get('MK_STAGE', '9'))
INORDER = bool(int(os.environ.get('MK_INORDER', '0')))
NTILE = 96
ECAP = 4096


class Stream:
    def __init__(self, sem, step, name):
        self.sem, self.step, self.count, self.name = sem, step, 0, name
        self.nobarrier = False


class Eng(Stream):
    def __init__(self, handle, sem, name, self_sync=True, speed=1000.0, fixed=0.08):
        super().__init__(sem, 1, name)
        self.h = handle
        self.seen = {}
        self.self_sync = self_sync
        self.speed = speed
        self.fixed = fixed
        self.free_t = 0.0


class Buf:
    __slots__ = ("w", "r", "name", "ps")

    def __init__(self, name="", ps=False):
        self.w, self.r, self.name, self.ps = set(), set(), name, ps


class Tl:
    def __init__(self, t, name=""):
        self.t = t
        self.b = Buf(name)


class Op:
    __slots__ = ("eng", "fns", "deps", "dur", "lat", "ds", "idx", "ticket", "stream", "done_t", "nsucc", "succ", "npend", "prio")

    def __init__(self, eng, fns, dur, ds=None, lat=0.0):
        self.eng, self.fns, self.dur, self.ds, self.lat = eng, fns, dur, ds, lat
        self.deps = set()
        self.ticket = None
        self.stream = None
        self.done_t = 0.0
        self.succ = []
        self.npend = 0
        self.prio = None


def _ap_elems(kw):
    ap = kw.get("out", None)
    if ap is None:
        ap = kw.get("ap", None)
    try:
        sh = ap.shape
        n = 1
        for d in sh[1:]:
            n *= d
        return n
    except Exception:
        return 512


class MK:
    def __init__(self, nc):
        self.nc = nc
        self.es = ExitStack()
        self.dsems = []
        self.pe = Eng(nc.tensor, self.sem("pe"), "pe", self_sync=False, speed=2400.0, fixed=0.03)
        self.act = Eng(nc.scalar, self.sem("act"), "act", speed=1000.0, fixed=0.2)
        self.dve = Eng(nc.vector, self.sem("dve"), "dve", speed=900.0, fixed=0.16)
        self.pool = Eng(nc.gpsimd, self.sem("pool"), "pool", speed=420.0, fixed=0.15)
        self.sp = Eng(nc.sync, self.sem("sp"), "sp")
        self.engs = [self.pe, self.act, self.dve, self.pool, self.sp]
        if int(os.environ.get("MK_NOSELF", "0")):
            for e_ in self.engs:
                e_.self_sync = False
        self.pending = []
        self.grp = None
        self.seg = None

    def begin_seg(self, slot):
        assert self.grp is None
        self.seg = (float(slot), [])

    def end_seg(self):
        assert self.grp is None
        slot, ops = self.seg
        n = max(len(ops), 1)
        if int(os.environ.get("MK_NOSEG", "0")):
            ops = []
        for k, o in enumerate(ops):
            o.prio = slot + (k + 0.5) / n
        self.seg = None

    def sem(self, name):
        return self.es.enter_context(self.nc.semaphore(name))

    def dsem(self, name):
        d = Stream(self.sem(name), 16, name)
        self.dsems.append(d)
        return d

    def sb(self, name, shape, dt, es=None):
        return (es or self.es).enter_context(self.nc.sbuf_tensor(name, shape, dt))

    def ps(self, name, shape, dt):
        return self.es.enter_context(self.nc.psum_tensor(name, shape, dt))

    def _record(self, op, reads, writes, par):
        for b in reads:
            op.deps |= b.w
            if b.ps:
                op.deps |= {r_ for r_ in b.r if r_.eng is not op.eng}
        for b in writes:
            if not par:
                op.deps |= b.w
            op.deps |= b.r
        op.deps.discard(op)
        for b in reads:
            b.r.add(op)
        for b in writes:
            if par:
                b.w = set(b.w)
                b.w.add(op)
            else:
                b.w = {op}
            b.r = set()

    def op(self, eng, fn, reads=(), writes=(), signal=True, dur=None, par=False):
        if dur is None:
            dur = eng.fixed + 512 / eng.speed
        if eng is self.pe:
            if self.grp is None:
                self.grp = (Op(eng, [], 0.0), set(), set())
            g, gr, gw = self.grp
            g.fns.append(fn)
            g.dur += dur
            gr.update(reads)
            gw.update(writes)
            if signal:
                self.grp = None
                self.pending.append(g)
                if self.seg is not None:
                    self.seg[1].append(g)
                self._record(g, gr, gw, par)
            return g
        o = Op(eng, [fn], dur)
        self.pending.append(o)
        if self.seg is not None:
            self.seg[1].append(o)
        self._record(o, reads, writes, par)
        return o

    def dma(self, eng, ds, fn, reads=(), writes=(), nbytes=1 << 20, par=False, lat=None):
        o = Op(eng, [fn], 0.15 if eng is not self.pool else 1.0, ds=ds, lat=(2.0 + nbytes / 150e3) if lat is None else lat)
        self.pending.append(o)
        if self.seg is not None:
            self.seg[1].append(o)
        self._record(o, reads, writes, par)
        return o

    def flush(self):
        assert self.grp is None
        ops = self.pending
        self.pending = []
        pend_set = set(ops)
        for i, o in enumerate(ops):
            o.idx = i
            o.deps = {d for d in o.deps if d in pend_set or d.ticket is not None}
        keep = set(os.environ.get("MK_KEEP", "").split(","))
        last_on = {}
        for o in ops:
            o.npend = 0
            for d in o.deps:
                if d in pend_set:
                    d.succ.append(o)
                    o.npend += 1
            if o.eng.name in keep:
                p = last_on.get(o.eng)
                if p is not None and p not in o.deps:
                    p.succ.append(o)
                    o.npend += 1
                last_on[o.eng] = o
        ready = [o for o in ops if o.npend == 0]
        t_base = max(e.free_t for e in self.engs)
        for e in self.engs:
            e.free_t = t_base
        nleft = len(ops)
        while nleft:
            best, bt = None, None
            for o in ready:
                st = o.eng.free_t
                for d in o.deps:
                    dt_ = d.done_t + (0.60 if d.eng is not o.eng else 0.08)
                    if dt_ > st:
                        st = dt_
                key = (st, o.idx) if not INORDER else ((o.prio if o.prio is not None else -1.0), o.idx)
                if bt is None or key < bt:
                    best, bt = o, key
            o = best
            ready.remove(o)
            st = bt[0]
            o.eng.free_t = st + o.dur
            o.done_t = st + o.dur + o.lat
            self._emit(o)
            nleft -= 1
            for s_ in o.succ:
                s_.npend -= 1
                if s_.npend == 0:
                    ready.append(s_)
            o.succ = []

    def _emit(self, o):
        eng = o.eng
        need = {}
        for d in o.deps:
            s, t = d.stream, d.ticket
            if need.get(s, 0) < t:
                need[s] = t
        for s, t in need.items():
            if s is eng and not eng.self_sync:
                continue
            if eng.seen.get(s, 0) >= t:
                continue
            eng.h.wait_ge(s.sem, t * s.step)
            eng.seen[s] = t
        inst = None
        for fn in o.fns:
            inst = fn()
        if o.ds is not None:
            o.ds.count += 1
            inst.then_inc(o.ds.sem, 16)
            o.stream, o.ticket = o.ds, o.ds.count
        else:
            eng.count += 1
            inst.then_inc(eng.sem, 1)
            o.stream, o.ticket = eng, eng.count
        o.deps = set()

    def barrier(self):
        self.flush()
        streams = list(self.engs) + [d for d in self.dsems if not getattr(d, "nobarrier", False)]
        for e in self.engs:
            for s in streams:
                if s.count == 0 or e.seen.get(s, 0) >= s.count:
                    continue
                e.h.wait_ge(s.sem, s.count * s.step)
                e.seen[s] = s.count


def declare_inputs(nc):
    I = {}

    def inp(name, shape, dt=F32):
        I[name] = nc.dram_tensor(name, shape, dt, kind="ExternalInput").ap()

    inp("x", [S, D])
    inp("c_pk", [128, 8])
    inp("pos_pj", [128, NT], I32)
    inp("w_ada", [D, 6 * D])
    inp("b_ada", [1, 6 * D])
    inp("norm_mix", [1, D])
    inp("w_in", [D, 1440])
    inp("conv_w_c", [128, 4, 31])
    inp("conv_b_c", [128, 4])
    inp("conv_ln_g_c", [128, 4])
    inp("conv_ln_b_c", [128, 4])
    inp("q_a_norm_c", [128, 2])
    inp("w_q_b", [256, 768])
    inp("kv_a_norm_c", [128, 1])
    inp("w_kv_b", [128, 1024])
    inp("q_norm", [1, 96])
    inp("k_norm", [1, 96])
    inp("w_out", [D, D])
    inp("norm_ffn", [1, D])
    inp("w_router", [D, 36])
    inp("b_router", [1, 36])
    inp("w_gate_e", [32, D, 256])
    inp("w_up_e", [32, D, 256])
    inp("w_down_e", [32, 256, D])
    return I


class _Stop(Exception):
    pass


def build(debug=None):
    holder = {}
    try:
        _build(debug, holder)
    except _Stop:
        holder["m"].es.close()
    return holder["nc"]


def _build(debug, holder):
    nc = bass.Bass("TRN2", target_bir_lowering=False)
    I = declare_inputs(nc)
    out_d = nc.dram_tensor("out", [S, D], F32, kind="ExternalOutput").ap()
    m = MK(nc)
    holder["nc"], holder["m"] = nc, m
    pe, act, dve, pool, sp = m.pe, m.act, m.dve, m.pool, m.sp

    def scr(name, shape, dt, dbg=False):
        kind = "ExternalOutput" if dbg else "Internal"
        return nc.dram_tensor(name, shape, dt, kind=kind).ap()

    dA = (debug or "").startswith("A")
    QT_d = scr("QT_d", [8, 96, S], BF16, dA)
    KT_d = scr("KT_d", [8, 96, S], BF16, dA)
    V_d = scr("V_d", [8, 128, NT, 64], BF16, dA)
    YC_d = scr("YC_d", [4, 128, S], BF16, dA)
    YA_d = scr("YA_d", [8, 64, S], BF16, debug == "B1")
    XS_d = scr("XS_d", [32 * ECAP, D], BF16)
    YS_d = scr("YS_d", [NTILE * 128, D], F32)
    WA_d = scr("WA_d", [32 * 128, 6144], BF16)
    WA3 = WA_d.rearrange("(e p) n -> e p n", p=128)
    B_QT, B_KT, B_V, B_YC, B_YA, B_XS, B_YS, B_WE, B_OUT = (Buf() for _ in range(9))

    psb = [Tl(m.ps(f"psb{i}", [128, 512], F32), f"psb{i}") for i in range(8)]
    for p_ in psb:
        p_.b.ps = True
    rr = {}

    def bank(role, banks):
        i = rr.get(role, 0)
        rr[role] = i + 1
        return psb[banks[i % len(banks)]]

    def tl(name, shape, dt, es=None):
        return Tl(m.sb(name, shape, dt, es), name)

    def O(eng, fname, reads=(), writes=(), signal=True, par=False, **kw):
        n = _ap_elems(kw)
        if eng is pe:
            mult = 4.0 if (kw.get("lhsT", kw.get("in_")).dtype == F32) else 1.0
            dur = eng.fixed + mult * max(n, 64) / eng.speed
        else:
            dur = eng.fixed + n / eng.speed
        return m.op(eng, lambda: getattr(eng.h, fname)(**kw), [r.b if isinstance(r, Tl) else r for r in reads],
                    [w.b if isinstance(w, Tl) else w for w in writes], signal, dur=dur, par=par)

    def DMA(eng, ds, out, in_, reads=(), writes=(), lat=None):
        return m.dma(eng, ds, lambda: eng.h.dma_start(out=out, in_=in_),
                     [r.b if isinstance(r, Tl) else r for r in reads],
                     [w.b if isinstance(w, Tl) else w for w in writes], lat=lat)

    ident_f = tl("ident_f", [128, 128], F32)
    ident_b = tl("ident_b", [128, 128], BF16)
    ones_f = tl("ones_f", [128, 128], F32)
    ones_b = tl("ones_b", [128, 128], BF16)

    negM = tl("negM", [128, 8], F32)
    O(pool, "memset", writes=[ident_f], ap=ident_f.t[:], constant=0.0)
    O(pool, "affine_select", reads=[ident_f], writes=[ident_f], out=ident_f.t[:], in_=ident_f.t[:],
      pattern=[[-1, 128]], compare_op=ALU.not_equal, fill=1.0, base=0, channel_multiplier=1)
    O(pool, "tensor_copy", reads=[ident_f], writes=[ident_b], out=ident_b.t[:], in_=ident_f.t[:])
    O(pool, "memset", writes=[ones_f], ap=ones_f.t[:], constant=1.0)
    O(pool, "memset", writes=[ones_b], ap=ones_b.t[:], constant=1.0)

    d_ld = [m.dsem(f"d_ld{i}") for i in range(4)]
    d_misc = m.dsem("d_misc")
    d_nrm = m.dsem("d_nrm")

    def adaln_parts(tag, es, banks, evac):
        d_c1, d_c2 = m.dsem("d_c1" + tag), m.dsem("d_c2" + tag)
        d_nrm_t = m.dsem("d_nrmA" + tag)
        c_sb = tl("c_sb" + tag, [128, 8], F32, es)
        cs = tl("cs" + tag, [128, 8], F32, es)
        csb = tl("csb" + tag, [128, 8, 128], F32, es)
        bada = tl("bada" + tag, [1, 6 * D], F32, es)
        wst = [tl(f"wst{i}" + tag, [128, 8, 512], F32, es) for i in range(2)]
        wsth = []
        for i in range(2):
            hv = []
            for hh in range(2):
                v = Tl(None, f"wst{i}h{hh}" + tag)
                v.t = wst[i].t[:, hh * 4:(hh + 1) * 4, :]
                hv.append(v)
            wsth.append(hv)
        nrm = tl("nrm" + tag, [128, D], F32, es)
        DMA(sp, d_c1, c_sb.t[:], I["c_pk"], writes=[c_sb])
        DMA(sp, d_c2, bada.t[:], I["b_ada"], writes=[bada])
        O(act, "activation", reads=[c_sb], writes=[cs], out=cs.t[:], in_=c_sb.t[:], func=AF.Silu)
        for k in range(8):
            O(dve, "tensor_scalar", reads=[cs, ones_f], writes=[csb], out=csb.t[:, k, :], in0=ones_f.t[:],
              scalar1=cs.t[:, k:k + 1], scalar2=None, op0=ALU.mult)
        wv = I["w_ada"].rearrange("(k p) n -> p k n", p=128)

        def chunk(j, dst, base, gate=(), wlat=None):
            wh = wsth[j % 2]
            for hh in range(2):
                DMA(sp if hh == 0 else act, d_ld[(j % 2) * 2 + hh], wh[hh].t,
                    wv[:, hh * 4:(hh + 1) * 4, j * 512:(j + 1) * 512], reads=gate, writes=[wh[hh]], lat=wlat)
            pb = bank("mm" + tag, banks)
            for k in range(8):
                O(pe, "matmul", reads=[csb, wh[k // 4]], writes=[pb], signal=False, out=pb.t[:], lhsT=csb.t[:, k, :],
                  rhs=wh[k // 4].t[:, k % 4, :], start=(k == 0), stop=False)
            O(pe, "matmul", reads=[ones_f, bada], writes=[pb], out=pb.t[:], lhsT=ones_f.t[0:1, :],
              rhs=bada.t[0:1, j * 512:(j + 1) * 512], start=False, stop=True)
            if evac is act:
                O(act, "copy", reads=[pb], writes=[dst], out=dst.t[:, (j - base) * 512:(j - base + 1) * 512], in_=pb.t[:])
            else:
                O(dve, "tensor_copy", reads=[pb], writes=[dst], out=dst.t[:, (j - base) * 512:(j - base + 1) * 512], in_=pb.t[:])

        def finish(dst, gsecs):
            for (gsec, nm) in gsecs:
                DMA(sp, d_nrm_t, nrm.t[:], I[nm].partition_broadcast(128), writes=[nrm])
                O(dve, "scalar_tensor_tensor", reads=[dst, nrm], writes=[dst], out=gsec, in0=gsec, scalar=1.0,
                  in1=nrm.t[:], op0=ALU.add, op1=ALU.mult)

        return chunk, finish

    def adaln(chunks, dst, base, gsecs, tag=""):
        with ExitStack() as es_own:
            chunk, finish = adaln_parts(tag, es_own, [2, 3], act)
            for j in chunks:
                chunk(j, dst, base)
            finish(dst, gsecs)
            m.barrier()

    if debug == "S0":
        m.es.close()
        return nc
    d_pc = m.dsem("d_pc")
    d_pc.nobarrier = True

    def precast(e, gate=()):
        m.dma(pool, d_pc, lambda: pool.h.dma_start(out=WA3[e][:, 0:2048], in_=I["w_gate_e"][e].rearrange("(p k) f -> p (k f)", k=8)),
              reads=gate, writes=[B_WE], par=True)
        m.dma(pool, d_pc, lambda: pool.h.dma_start(out=WA3[e][:, 2048:4096], in_=I["w_up_e"][e].rearrange("(p k) f -> p (k f)", k=8)),
              reads=gate, writes=[B_WE], par=True)
        m.dma(pool, d_pc, lambda: pool.h.dma_start(out=WA3[e][:, 4096:6144].rearrange("p (j n) -> p j n", j=2),
                                                   in_=I["w_down_e"][e].rearrange("(j p) n -> p j n", p=128)),
              reads=gate, writes=[B_WE], par=True)

    with ExitStack() as es:
        modA = tl("modA", [128, 2 * D], F32, es)
        SH_A, G_A = modA.t[:, 0:D], modA.t[:, D:2 * D]
        win = tl("win", [128, 8, 1440], BF16, es)
        wqb = tl("wqb", [128, 2, 768], BF16, es)
        wkvb = tl("wkvb", [128, 1024], BF16, es)
        diag = tl("diag", [128, 4, 31, 128], BF16, es)
        cw = tl("cw", [128, 4, 31], F32, es)
        cb = tl("cb", [128, 4], F32, es)
        lng = tl("lng", [128, 4], F32, es)
        lnb = tl("lnb", [128, 4], F32, es)
        gq = tl("gq", [128, 8, 96], F32, es)
        gk = tl("gk", [128, 8, 96], F32, es)
        cos_t = tl("cos_t", [128, NT, 16], F32, es)
        sin_t = tl("sin_t", [128, NT, 16], F32, es)
        avg_f = tl("avg_f", [128, 128], F32, es)
        d_w = m.dsem("d_w")
        m.dma(pool, d_w, lambda: pool.h.dma_start(out=win.t[:], in_=I["w_in"].rearrange("(k p) n -> p k n", p=128)),
              writes=[win.b])
        with ExitStack() as es2:
            wq_st = tl("wq_st", [128, 2, 768], F32, es2)
            wkv_st = tl("wkv_st", [128, 1024], F32, es2)
            qan = tl("qan", [128, 2], F32, es2)
            kvan = tl("kvan", [128, 1], F32, es2)
            gtmp = tl("gtmp", [128, 96], F32, es2)
            posi = tl("posi", [128, NT], I32, es2)
            posf = tl("posf", [128, NT], F32, es2)
            ang = tl("ang", [128, NT, 16], F32, es2)
            rk = tl("rk", [128, NT, 16], F32, es2)
            rki = tl("rki", [128, NT, 16], I32, es2)
            rr_ = tl("rr_", [128, NT, 16], F32, es2)
            msk = tl("msk", [128, NT, 16], F32, es2)
            dm = [m.dsem(f"d_misc{i}") for i in range(9)]
            DMA(sp, dm[0], wq_st.t[:], I["w_q_b"].rearrange("(k p) n -> p k n", p=128), writes=[wq_st])
            DMA(sp, dm[1], wkv_st.t[:], I["w_kv_b"], writes=[wkv_st])
            DMA(sp, dm[2], qan.t[:], I["q_a_norm_c"], writes=[qan])
            DMA(sp, dm[3], kvan.t[:], I["kv_a_norm_c"], writes=[kvan])
            DMA(sp, dm[4], cw.t[:], I["conv_w_c"], writes=[cw])
            DMA(sp, dm[5], cb.t[:], I["conv_b_c"], writes=[cb])
            DMA(sp, dm[6], lng.t[:], I["conv_ln_g_c"], writes=[lng])
            DMA(sp, dm[7], lnb.t[:], I["conv_ln_b_c"], writes=[lnb])
            DMA(sp, dm[8], posi.t[:], I["pos_pj"], writes=[posi])
            for k in range(2):
                O(dve, "tensor_scalar", reads=[wq_st, qan], writes=[wqb], out=wqb.t[:, k, :], in0=wq_st.t[:, k, :],
                  scalar1=qan.t[:, k:k + 1], scalar2=None, op0=ALU.mult)
            O(dve, "tensor_scalar", reads=[wkv_st, kvan], writes=[wkvb], out=wkvb.t[:], in0=wkv_st.t[:],
              scalar1=kvan.t[:, 0:1], scalar2=None, op0=ALU.mult)
            for (g, nm, sc) in ((gq, "q_norm", 96.0 ** -0.5), (gk, "k_norm", 1.0)):
                DMA(sp, d_nrm, gtmp.t[:], I[nm].partition_broadcast(128), writes=[gtmp])
                for h in range(8):
                    O(dve, "tensor_scalar", reads=[gtmp], writes=[g], out=g.t[:, h, :], in0=gtmp.t[:], scalar1=sc,
                      scalar2=None, op0=ALU.mult)
            for ci, g in enumerate((gq, gk)):
                c0 = 4 + 2 * ci
                O(dve, "tensor_reduce", reads=[g], writes=[negM], out=negM.t[:, c0:c0 + 1], in_=g.t[:, 0, :], axis=AX.X, op=ALU.max)
                O(dve, "tensor_reduce", reads=[g], writes=[negM], out=negM.t[:, c0 + 1:c0 + 2], in_=g.t[:, 0, :], axis=AX.X, op=ALU.min)
                O(dve, "tensor_scalar", reads=[negM], writes=[negM], out=negM.t[:, c0 + 1:c0 + 2], in0=negM.t[:, c0 + 1:c0 + 2], scalar1=-1.0,
                  scalar2=None, op0=ALU.mult)
                O(dve, "tensor_tensor", reads=[negM], writes=[negM], out=negM.t[:, 1 + ci:2 + ci], in0=negM.t[:, c0:c0 + 1],
                  in1=negM.t[:, c0 + 1:c0 + 2], op=ALU.max)
            O(dve, "tensor_tensor", reads=[negM], writes=[negM], out=negM.t[:, 3:4], in0=negM.t[:, 1:2], in1=negM.t[:, 2:3], op=ALU.mult)
            O(dve, "tensor_scalar", reads=[negM], writes=[negM], out=negM.t[:, 0:1], in0=negM.t[:, 3:4], scalar1=-96.0, scalar2=None, op0=ALU.mult)
            for j in range(4):
                for k in range(31):
                    if (j * 31 + k) % 2 == 0:
                        O(dve, "tensor_scalar", reads=[ident_f, cw], writes=[diag], par=True, out=diag.t[:, j, k, :], in0=ident_f.t[:],
                          scalar1=cw.t[:, j, k:k + 1], scalar2=None, op0=ALU.mult)
                    else:
                        O(act, "activation", reads=[ident_f, cw], writes=[diag], par=True, out=diag.t[:, j, k, :], in_=ident_f.t[:],
                          func=AF.Identity, scale=cw.t[:, j, k:k + 1])
            O(pool, "memset", writes=[avg_f], ap=avg_f.t[:], constant=1.0 / 512.0)
            O(dve, "tensor_copy", reads=[posi], writes=[posf], out=posf.t[:], in_=posi.t[:])
            for i in range(16):
                O(dve, "tensor_scalar", reads=[posf], writes=[ang], out=ang.t[:, :, i], in0=posf.t[:],
                  scalar1=float(np.float32(10000.0) ** np.float32(-(2.0 * i) / 32.0)), scalar2=None, op0=ALU.mult)
            C1 = 6.28125
            C2 = 2.0 * math.pi - C1
            for (tab, shift) in ((sin_t, 0.0), (cos_t, math.pi / 2.0)):
                O(dve, "tensor_scalar", reads=[ang], writes=[rk], out=rk.t[:], in0=ang.t[:], scalar1=shift,
                  scalar2=1.0 / (2.0 * math.pi), op0=ALU.add, op1=ALU.mult)
                O(dve, "tensor_copy", reads=[rk], writes=[rki], out=rki.t[:], in_=rk.t[:])
                O(dve, "tensor_copy", reads=[rki], writes=[rk], out=rk.t[:], in_=rki.t[:])
                O(dve, "scalar_tensor_tensor", reads=[rk, ang], writes=[rr_], out=rr_.t[:], in0=rk.t[:], scalar=-C1,
                  in1=ang.t[:], op0=ALU.mult, op1=ALU.add)
                O(dve, "scalar_tensor_tensor", reads=[rk, rr_], writes=[rr_], out=rr_.t[:], in0=rk.t[:], scalar=-C2,
                  in1=rr_.t[:], op0=ALU.mult, op1=ALU.add)
                if shift:
                    O(dve, "tensor_scalar", reads=[rr_], writes=[rr_], out=rr_.t[:], in0=rr_.t[:], scalar1=shift,
                      scalar2=None, op0=ALU.add)
                O(dve, "tensor_scalar", reads=[rr_], writes=[msk], out=msk.t[:], in0=rr_.t[:], scalar1=math.pi,
                  scalar2=-2.0 * math.pi, op0=ALU.is_gt, op1=ALU.mult)
                O(dve, "tensor_tensor", reads=[rr_, msk], writes=[rr_], out=rr_.t[:], in0=rr_.t[:], in1=msk.t[:], op=ALU.add)
                O(dve, "tensor_scalar", reads=[rr_], writes=[msk], out=msk.t[:], in0=rr_.t[:], scalar1=-math.pi,
                  scalar2=2.0 * math.pi, op0=ALU.is_lt, op1=ALU.mult)
                O(dve, "tensor_tensor", reads=[rr_, msk], writes=[rr_], out=rr_.t[:], in0=rr_.t[:], in1=msk.t[:], op=ALU.add)
                O(dve, "tensor_scalar", reads=[rr_], writes=[rr_], out=rr_.t[:], in0=rr_.t[:], scalar1=math.pi,
                  scalar2=-math.pi, op0=ALU.min, op1=ALU.max)
                O(act, "activation", reads=[rr_], writes=[tab], out=tab.t[:], in_=rr_.t[:], func=AF.Sin)
            adaln(range(0, 4), modA, 0, [(G_A, "norm_mix")])

        if debug == "S1":
            es.close()
            m.es.close()
            return
        xt = [tl(f"xt{i}", [128, D], F32, es) for i in range(3)]
        junk = tl("junk", [128, D], F32, es)
        hmid = tl("hmid", [128, D], F32, es)
        hb = [tl(f"hb{i}", [128, D], BF16, es) for i in range(2)]
        hTs = [tl(f"hT{i}", [128, 8, 512], BF16, es) for i in range(2)]
        st = [tl(f"st{i}", [128, 8], F32, es) for i in range(8)]
        junk4 = tl("junk4", [128, 256], F32, es)
        ub = [tl(f"ub{i}", [128, 4, 542], BF16, es) for i in range(2)]
        sig = [tl(f"sig{i}", [128, 512], F32, es) for i in range(2)]
        cqTs = [tl(f"cqT{i}", [128, 2, 512], BF16, es) for i in range(2)]
        ckvTs = [tl(f"ckvT{i}", [128, 512], BF16, es) for i in range(2)]
        dwb = tl("dwb", [128, 4, 512], F32, es)
        dw2 = [tl(f"dw2{i}", [128, 512], F32, es) for i in range(4)]
        mean = tl("mean", [128, 512], F32, es)
        rstd = tl("rstd", [128, 512], F32, es)
        lt = [tl(f"lt{i}", [128, 512], F32, es) for i in range(2)]
        yc = tl("yc", [128, 4, 512], BF16, es)
        sq = tl("sq", [128, 8, 96], F32, es)
        qn = tl("qn", [128, 8, 96], F32, es)
        kf = tl("kf", [128, 8, 96], F32, es)
        kn = tl("kn", [128, 8, 96], F32, es)
        rt = [tl(f"rt{i}", [128, 8, 16], F32, es) for i in range(4)]
        qfin = [tl(f"qfin{i}", [128, 8, 96], BF16, es) for i in range(2)]
        kfin = [tl(f"kfin{i}", [128, 8, 96], BF16, es) for i in range(2)]
        vblk = tl("vblk", [128, 8, 4, 64], BF16, es)
        qTb = tl("qTb", [128, 8, 512], BF16, es)
        kTb = tl("kTb", [128, 8, 512], BF16, es)
        d_x = [m.dsem(f"d_x{i}") for i in range(3)]
        d_sty, d_stq, d_stk, d_stv = (m.dsem(n) for n in ("d_sty", "d_stq", "d_stk", "d_stv"))

        for i in range(2):
            O(pool, "memset", writes=[ub[i]], ap=ub[i].t[:, :, 0:30], constant=0.0)

        def load_x(t):
            DMA(sp, d_x[t % 3], xt[t % 3].t[:], I["x"][t * 128:(t + 1) * 128, :], writes=[xt[t % 3]])

        load_x(0)
        load_x(1)
        pc_next = 0
        for b in range(NB if not (debug or "").startswith("A") or len(debug) == 1 else int(debug[1:])):
            for _ in range(4 if not int(os.environ.get("MK_NOPC", "0")) else 0):
                if pc_next < 32:
                    precast(pc_next, gate=[hTs[(b + 1) % 2].b] if b > 0 else ())
                    pc_next += 1
            m.begin_seg(b)
            u = ub[b % 2]
            up = ub[(b + 1) % 2]
            hT, cqT, ckvT = hTs[b % 2], cqTs[b % 2], ckvTs[b % 2]
            for r in range(4):
                t = b * 4 + r
                x = xt[t % 3]
                if t + 2 < NT:
                    load_x(t + 2)
                s = st[t % 8]
                O(act, "activation", reads=[x], writes=[junk, s], out=junk.t[:], in_=x.t[:], func=AF.Square,
                  accum_out=s.t[:, 0:1])
                O(act, "activation", reads=[s], writes=[s], out=s.t[:, 1:2], in_=s.t[:, 0:1], func=AF.Sqrt,
                  scale=1.0 / D, bias=EPS)
                O(dve, "reciprocal", reads=[s], writes=[s], out=s.t[:, 2:3], in_=s.t[:, 1:2])
                O(dve, "scalar_tensor_tensor", reads=[x, s, modA], writes=[hmid], out=hmid.t[:], in0=x.t[:],
                  scalar=s.t[:, 2:3], in1=G_A, op0=ALU.mult, op1=ALU.mult)
                h = hb[t % 2]
                O(pool, "tensor_tensor", reads=[hmid, modA], writes=[h], out=h.t[:], in0=hmid.t[:], in1=SH_A, op=ALU.add)
                pb = bank("tpA", [0])
                pbv = pb.t[:].bitcast(BF16).rearrange("p (k n) -> p k n", k=8)
                for k in range(8):
                    O(pe, "transpose", reads=[h, ident_b], writes=[pb], signal=(k == 7), out=pbv[:, k, :],
                      in_=h.t[:, k * 128:(k + 1) * 128], identity=ident_b.t[:])
                O(act if r % 2 == 0 else dve, "copy" if r % 2 == 0 else "tensor_copy", reads=[pb], writes=[hT],
                  out=hT.t[:, :, r * 128:(r + 1) * 128], in_=pbv)
            for j in range(4):
                pg = bank("mmA", [2, 3])
                for k in range(8):
                    O(pe, "matmul", reads=[win, hT], writes=[pg], signal=(k == 7), out=pg.t[:],
                      lhsT=win.t[:, k, 512 + j * 128:512 + (j + 1) * 128], rhs=hT.t[:, k, :], start=(k == 0), stop=(k == 7))
                sg = sig[j % 2]
                O(act, "activation", reads=[pg], writes=[sg], out=sg.t[:], in_=pg.t[:], func=AF.Sigmoid)
                pv = bank("mmA", [2, 3])
                for k in range(8):
                    O(pe, "matmul", reads=[win, hT], writes=[pv], signal=(k == 7), out=pv.t[:],
                      lhsT=win.t[:, k, j * 128:(j + 1) * 128], rhs=hT.t[:, k, :], start=(k == 0), stop=(k == 7))
                O(dve, "tensor_tensor", reads=[pv, sg], writes=[u], out=u.t[:, j, 30:542], in0=pv.t[:], in1=sg.t[:], op=ALU.mult)
            if b + 1 < NB:
                O(pool, "tensor_copy", reads=[u], writes=[up], out=up.t[:, :, 0:30], in_=u.t[:, :, 512:542])
            for j in range(3):
                pq = bank("mmA", [2, 3])
                for k in range(8):
                    O(pe, "matmul", reads=[win, hT], writes=[pq], signal=(k == 7), out=pq.t[:],
                      lhsT=win.t[:, k, 1024 + j * 128:1024 + (j + 1) * 128], rhs=hT.t[:, k, :], start=(k == 0), stop=(k == 7))
                if j < 2:
                    O(act, "copy", reads=[pq], writes=[cqT], out=cqT.t[:, j, :], in_=pq.t[:])
                else:
                    O(act, "copy", reads=[pq], writes=[ckvT], out=ckvT.t[:], in_=pq.t[:])
            pm = bank("stat", [6, 7])
            pm2 = bank("stat", [6, 7])
            for j in range(4):
                pc = bank("mmA", [2, 3])
                for k in range(31):
                    O(pe, "matmul", reads=[diag, u], writes=[pc], signal=(k == 30), out=pc.t[:], lhsT=diag.t[:, j, k, :],
                      rhs=u.t[:, j, k:k + 512], start=(k == 0), stop=(k == 30))
                O(act, "activation", reads=[pc, cb], writes=[dwb], out=dwb.t[:, j, :], in_=pc.t[:], func=AF.Identity,
                  bias=cb.t[:, j:j + 1])
                d2 = dw2[j]
                O(act, "activation", reads=[pc, cb], writes=[d2], out=d2.t[:], in_=pc.t[:], func=AF.Square,
                  bias=cb.t[:, j:j + 1])
            for j in range(4):
                O(pe, "matmul", reads=[avg_f, dwb], writes=[pm], signal=(j == 3), out=pm.t[:], lhsT=avg_f.t[:],
                  rhs=dwb.t[:, j, :], start=(j == 0), stop=(j == 3))
            for j in range(4):
                O(pe, "matmul", reads=[avg_f, dw2[j]], writes=[pm2], signal=(j == 3), out=pm2.t[:], lhsT=avg_f.t[:],
                  rhs=dw2[j].t[:], start=(j == 0), stop=(j == 3))
            O(act, "copy", reads=[pm], writes=[mean], out=mean.t[:], in_=pm.t[:])
            O(dve, "tensor_tensor", reads=[mean], writes=[rstd], out=rstd.t[:], in0=mean.t[:], in1=mean.t[:], op=ALU.mult)
            O(dve, "tensor_tensor", reads=[pm2, rstd], writes=[rstd], out=rstd.t[:], in0=pm2.t[:], in1=rstd.t[:], op=ALU.subtract)
            O(act, "activation", reads=[rstd], writes=[rstd], out=rstd.t[:], in_=rstd.t[:], func=AF.Sqrt, bias=EPS)
            O(dve, "reciprocal", reads=[rstd], writes=[rstd], out=rstd.t[:], in_=rstd.t[:])
            for j in range(4):
                l = lt[j % 2]
                O(dve, "tensor_tensor", reads=[dwb, mean], writes=[l], out=l.t[:], in0=dwb.t[:, j, :], in1=mean.t[:], op=ALU.subtract)
                O(pool, "tensor_tensor", reads=[l, rstd], writes=[l], out=l.t[:], in0=l.t[:], in1=rstd.t[:], op=ALU.mult)
                O(act, "activation", reads=[l, lng, lnb], writes=[yc], out=yc.t[:, j, :], in_=l.t[:], func=AF.Silu,
                  scale=lng.t[:, j:j + 1], bias=lnb.t[:, j:j + 1])
            DMA(sp, d_sty, YC_d[:, :, b * 512:(b + 1) * 512].rearrange("j p n -> p j n"), yc.t[:], reads=[yc], writes=[B_YC])
            m.end_seg()
            m.begin_seg(b + 1)
            for r in range(4):
                t = b * 4 + r
                s = st[t % 8]
                tok = slice(r * 128, (r + 1) * 128)
                pt = bank("mmB", [4, 5])
                for k in range(8):
                    O(pe, "matmul", reads=[win, hT], writes=[pt], signal=(k == 7), out=pt.t[:, 0:416], lhsT=hT.t[:, k, tok],
                      rhs=win.t[:, k, 1024:1440], start=(k == 0), stop=(k == 7))
                O(act, "activation", reads=[pt], writes=[junk4, s], out=junk4.t[:, 0:256], in_=pt.t[:, 0:256], func=AF.Square,
                  accum_out=s.t[:, 3:4])
                O(act, "activation", reads=[pt], writes=[junk4, s], out=junk4.t[:, 0:128], in_=pt.t[:, 256:384], func=AF.Square,
                  accum_out=s.t[:, 4:5])
                O(act, "activation", reads=[s], writes=[s], out=s.t[:, 5:6], in_=s.t[:, 3:4], func=AF.Sqrt, scale=1.0 / 256, bias=EPS)
                O(act, "activation", reads=[s], writes=[s], out=s.t[:, 6:7], in_=s.t[:, 4:5], func=AF.Sqrt, scale=1.0 / 128, bias=EPS)
                O(dve, "reciprocal", reads=[s], writes=[s], out=s.t[:, 5:7], in_=s.t[:, 5:7])
                O(act, "copy", reads=[pt], writes=[kf], out=kf.t[:, :, 64:96],
                  in_=pt.t[:, 384:416].unsqueeze(1).to_broadcast([128, 8, 32]))
                pq0 = bank("mmB", [4, 5])
                pq1 = bank("mmB", [4, 5])
                for n, pq in enumerate((pq0, pq1)):
                    for k in range(2):
                        O(pe, "matmul", reads=[cqT, wqb], writes=[pq], signal=(k == 1), out=pq.t[:, 0:384], lhsT=cqT.t[:, k, tok],
                          rhs=wqb.t[:, k, n * 384:(n + 1) * 384], start=(k == 0), stop=(k == 1))
                for n, pq in enumerate((pq0, pq1)):
                    O(act, "activation", reads=[pq, s], writes=[qn], out=qn.t[:, n * 4:(n + 1) * 4, :],
                      in_=pq.t[:, 0:384].rearrange("p (h d) -> p h d", h=4), func=AF.Identity, scale=s.t[:, 5:6])
                pk0 = bank("mmB", [4, 5])
                pk1 = bank("mmB", [4, 5])
                for n, pk in enumerate((pk0, pk1)):
                    O(pe, "matmul", reads=[ckvT, wkvb], writes=[pk], out=pk.t[:], lhsT=ckvT.t[:, tok],
                      rhs=wkvb.t[:, n * 512:(n + 1) * 512], start=True, stop=True)
                for n, pk in enumerate((pk0, pk1)):
                    pkv = pk.t[:].rearrange("p (h d) -> p h d", h=4)
                    O(act, "activation", reads=[pk, s], writes=[kf], out=kf.t[:, n * 4:(n + 1) * 4, 0:64], in_=pkv[:, :, 0:64],
                      func=AF.Identity, scale=s.t[:, 6:7])
                    O(dve, "tensor_scalar", reads=[pk, s], writes=[vblk], out=vblk.t[:, n * 4:(n + 1) * 4, r, :], in0=pkv[:, :, 64:128],
                      scalar1=s.t[:, 6:7], scalar2=None, op0=ALU.mult)
                for (src, dst, g, fin, so) in ((qn, qn, gq, qfin[t % 2], 0), (kf, kn, gk, kfin[t % 2], 1)):
                    ss = st[t % 8]
                    eng2 = dve if so == 0 else pool
                    O(eng2, "tensor_tensor", reads=[src], writes=[sq], out=sq.t[:], in0=src.t[:], in1=src.t[:], op=ALU.mult)
                    O(dve, "tensor_reduce", reads=[sq], writes=[rt[3]], out=rt[3].t[:, :, so], in_=sq.t[:], axis=AX.X, op=ALU.add)
                    O(act, "activation", reads=[rt[3]], writes=[rt[3]], out=rt[3].t[:, :, 2 + so], in_=rt[3].t[:, :, so], func=AF.Sqrt,
                      scale=1.0 / 96, bias=EPS)
                    O(dve, "reciprocal", reads=[rt[3]], writes=[rt[3]], out=rt[3].t[:, :, 4 + so], in_=rt[3].t[:, :, 2 + so])
                    O(dve, "tensor_tensor", reads=[src, rt[3]], writes=[dst], out=dst.t[:], in0=src.t[:],
                      in1=rt[3].t[:, :, 4 + so:5 + so].to_broadcast([128, 8, 96]), op=ALU.mult)
                    O(eng2, "tensor_tensor", reads=[dst, g], writes=[dst], out=dst.t[:], in0=dst.t[:], in1=g.t[:], op=ALU.mult)
                    O(act, "copy", reads=[dst], writes=[fin], out=fin.t[:, :, 0:64], in_=dst.t[:, :, 0:64])
                    cosb = cos_t.t[:, t:t + 1, :].to_broadcast([128, 8, 16])
                    sinb = sin_t.t[:, t:t + 1, :].to_broadcast([128, 8, 16])
                    x1 = dst.t[:, :, 64:80]
                    x2 = dst.t[:, :, 80:96]
                    O(pool, "tensor_tensor", reads=[dst, cos_t], writes=[rt[0]], out=rt[0].t[:], in0=x1, in1=cosb, op=ALU.mult)
                    O(pool, "tensor_tensor", reads=[dst, sin_t], writes=[rt[1]], out=rt[1].t[:], in0=x2, in1=sinb, op=ALU.mult)
                    O(pool, "tensor_tensor", reads=[rt[0], rt[1]], writes=[fin], out=fin.t[:, :, 64:80], in0=rt[0].t[:], in1=rt[1].t[:],
                      op=ALU.subtract)
                    O(dve, "tensor_tensor", reads=[dst, sin_t], writes=[rt[0]], out=rt[0].t[:], in0=x1, in1=sinb, op=ALU.mult)
                    O(dve, "tensor_tensor", reads=[dst, cos_t], writes=[rt[1]], out=rt[1].t[:], in0=x2, in1=cosb, op=ALU.mult)
                    O(dve, "tensor_tensor", reads=[rt[0], rt[1]], writes=[fin], out=fin.t[:, :, 80:96], in0=rt[0].t[:], in1=rt[1].t[:],
                      op=ALU.add)
                for (fin, dstT) in ((qfin[t % 2], qTb), (kfin[t % 2], kTb)):
                    pb = bank("tpB", [1])
                    pbv = pb.t[:].bitcast(BF16).rearrange("p (k n) -> p k n", k=8)
                    for h in range(8):
                        O(pe, "transpose", reads=[fin, ident_b], writes=[pb], signal=(h == 7), out=pbv[0:96, h, :],
                          in_=fin.t[:, h, :], identity=ident_b.t[:])
                    O(dve, "tensor_copy", reads=[pb], writes=[dstT], out=dstT.t[0:96, :, tok], in_=pbv[0:96, :, :])
            DMA(sp, d_stq, QT_d[:, :, b * 512:(b + 1) * 512].rearrange("h p n -> p h n"), qTb.t[0:96, :, :], reads=[qTb], writes=[B_QT])
            DMA(act, d_stk, KT_d[:, :, b * 512:(b + 1) * 512].rearrange("h p n -> p h n"), kTb.t[0:96, :, :], reads=[kTb], writes=[B_KT])
            DMA(sp, d_stv, V_d[:, :, b * 4:(b + 1) * 4, :].rearrange("h p j d -> p h j d"), vblk.t[:], reads=[vblk], writes=[B_V])
            m.end_seg()
        while pc_next < 32:
            precast(pc_next)
            pc_next += 1
        m.barrier()

    if dA:
        m.es.close()
        return nc

    modB = tl("modB", [128, 4 * D], F32)
    GT_A, SH_F, G_F, GT_F = (modB.t[:, i * D:(i + 1) * D] for i in range(4))
    d_wo = m.dsem("d_wo")
    wout = tl("wout", [128, 8, 1024], BF16)
    m.dma(pool, d_wo, lambda: pool.h.dma_start(out=wout.t[:], in_=I["w_out"].rearrange("(k p) n -> p k n", p=128)),
          writes=[wout.b])
    with ExitStack() as es:
        ada_chunk, ada_finish = adaln_parts("_b", es, [0, 1], dve)
        QTh = [tl(f"QTh{i}", [128, S], BF16, es) for i in range(2)]
        KTh = [tl(f"KTh{i}", [128, S], BF16, es) for i in range(2)]
        Vh = [tl(f"Vh{i}", [128, NT, 128], BF16, es) for i in range(2)]
        PT = [tl(f"PT{i}", [128, 512], BF16, es) for i in range(6)]
        PTd = [tl(f"PTd{i}", [128, 512], BF16, es) for i in range(8)]
        Un = [tl(f"Un{i}", [128, 512], F32, es) for i in range(2)]
        Rc = [tl(f"Rc{i}", [128, 512], F32, es) for i in range(2)]
        R2 = [tl(f"R2{i}", [128, 512], F32, es) for i in range(2)]
        Yh = [tl(f"Yh{i}", [128, 512], BF16, es) for i in range(2)]
        d_q = [m.dsem(f"d_q{i}") for i in range(2)]
        d_k = [m.dsem(f"d_k{i}") for i in range(2)]
        d_v = [m.dsem(f"d_v{i}") for i in range(2)]
        d_r2 = [m.dsem(f"d_r2{i}") for i in range(2)]
        d_ya = [m.dsem(f"d_ya{i}") for i in range(2)]
        for i in range(2):
            O(pool, "memset", writes=[Vh[i]], ap=Vh[i].t[:, :, 64:128], constant=1.0)
            O(pool, "memset", writes=[KTh[i]], ap=KTh[i].t[64:128, :], constant=1.0)
            O(dve, "tensor_scalar", reads=[KTh[i], negM], writes=[QTh[i]], out=QTh[i].t[64:128, :], in0=KTh[i].t[64:128, :],
              scalar1=negM.t[64:128, 0:1], scalar2=None, op0=ALU.mult)

        def load_head(h):
            i = h % 2
            DMA(sp, d_q[i], QTh[i].t[0:96, :], QT_d[h], reads=[B_QT], writes=[QTh[i]])
            DMA(sp, d_k[i], KTh[i].t[0:96, :], KT_d[h], reads=[B_KT], writes=[KTh[i]])
            DMA(act, d_v[i], Vh[i].t[:, :, 0:64], V_d[h], reads=[B_V], writes=[Vh[i]])

        load_head(0)
        nq = 0
        for h in range(8):
            if h + 1 < 8:
                load_head(h + 1)
            Q, Kt, V = QTh[h % 2], KTh[h % 2], Vh[h % 2]
            for qb in range(NB):
                if h == 0:
                    ada_chunk(4 + qb, modB, 4, gate=[Yh[(nq + 1) % 2]] if qb > 0 else (), wlat=40.0)
                    if qb == NB - 1:
                        ada_finish(modB, [(G_F, "norm_ffn")])
                po = bank("o", [6, 7])
                njt = 4 * qb + 4
                pend = []

                def emit_s(j):
                    r = j - 4 * qb
                    c0 = 128 * max(r, 0)
                    n = 512 - c0
                    ps = bank("s", [2, 3, 4, 5])
                    O(pe, "matmul", reads=[Kt, Q], writes=[ps], out=ps.t[:, 0:n], lhsT=Kt.t[0:97, j * 128:(j + 1) * 128],
                      rhs=Q.t[0:97, qb * 512 + c0:(qb + 1) * 512], start=True, stop=True)
                    if r >= 0:
                        pt = PTd[(4 * (nq % 2)) + r]
                        O(act, "activation", reads=[ps], writes=[pt], out=pt.t[:, 0:n], in_=ps.t[:, 0:n], func=AF.Exp)
                        O(pool, "memset", writes=[pt], ap=pt.t[64:128, 0:64], constant=0.0)
                    else:
                        pt = bank_pt()
                        O(act, "activation", reads=[ps], writes=[pt], out=pt.t[:, 0:n], in_=ps.t[:, 0:n], func=AF.Exp)
                    return (j, pt, c0, n)

                def bank_pt():
                    i = rr.get("pt", 0)
                    rr["pt"] = i + 1
                    return PT[i % 6]

                def emit_pv(item):
                    j, pt, c0, n = item
                    O(pe, "matmul", reads=[V, pt], writes=[po], signal=(j == njt - 1), out=po.t[:, c0:512], lhsT=V.t[:, j, :],
                      rhs=pt.t[:, 0:n], start=(j == 0), stop=(j == njt - 1))

                LOOK = 3
                for j in range(njt):
                    pend.append(emit_s(j))
                    if len(pend) > LOOK:
                        emit_pv(pend.pop(0))
                while pend:
                    emit_pv(pend.pop(0))
                i = nq % 2
                nq += 1
                O(dve, "tensor_copy", reads=[po], writes=[Un[i]], out=Un[i].t[0:64, :], in_=po.t[0:64, :])
                O(dve, "reciprocal", reads=[po], writes=[Rc[i]], out=Rc[i].t[64:128, :], in_=po.t[64:128, :])
                DMA(sp, d_r2[i], R2[i].t[0:64, :], Rc[i].t[64:128, :], reads=[Rc[i]], writes=[R2[i]])
                O(pool, "tensor_tensor", reads=[Un[i], R2[i]], writes=[Yh[i]], out=Yh[i].t[0:64, :], in0=Un[i].t[0:64, :],
                  in1=R2[i].t[0:64, :], op=ALU.mult)
                DMA(sp, d_ya[i], YA_d[h, :, qb * 512:(qb + 1) * 512], Yh[i].t[0:64, :], reads=[Yh[i]], writes=[B_YA])
        m.barrier()

    if debug == "B1":
        m.es.close()
        return nc

    SP_E = [mybir.EngineType.SP]
    OH = tl("OH", [128, NT, 2, 32], F32)
    Wt = tl("Wt", [128, NT, 2], F32)
    PF = tl("PF", [128, NT, 2], F32)
    Ssum = tl("Ssum", [128, 32], F32)
    EOFF = tl("EOFF", [128, 32], F32)
    ustr = tl("ustr", [128, 128], F32)
    O(pool, "memset", writes=[Ssum], ap=Ssum.t[:], constant=0.0)
    for e in range(32):
        O(pool, "memset", writes=[EOFF], ap=EOFF.t[:, e:e + 1], constant=float(e * ECAP))
    O(pool, "memset", writes=[ustr], ap=ustr.t[:], constant=1.0)
    O(pool, "affine_select", reads=[ustr], writes=[ustr], out=ustr.t[:], in_=ustr.t[:], pattern=[[1, 128]],
      compare_op=ALU.is_gt, fill=0.0, base=0, channel_multiplier=-1)
    with ExitStack() as es:
        wr = tl("wr", [128, 8, 36], F32, es)
        br = tl("br", [1, 36], F32, es)
        ycat = [tl(f"ycat{i}", [128, 8, 512], BF16, es) for i in range(2)]
        xr = [tl(f"xr{i}", [128, D], F32, es) for i in range(2)]
        x1 = [tl(f"x1{i}", [128, D], F32, es) for i in range(2)]
        h2b = [tl(f"h2b{i}", [128, D], BF16, es) for i in range(2)]
        sm = [tl(f"sm{i}", [128, 16], F32, es) for i in range(2)]
        tmp2 = {}
        for nm_, sh_ in (("junkb", [128, D]), ("hm", [128, D]), ("h2f", [128, D]), ("h2T", [128, 8, 128]), ("L", [128, 36]),
                         ("goh", [128, 4]), ("gex", [128, 4]), ("t48", [128, 4, 8]), ("ein", [128, 8]), ("oh1", [128, 8]),
                         ("oh2", [128, 8]), ("msk8", [128, 8]), ("St", [128, 32]), ("Cb", [128, 32]), ("t32", [128, 32])):
            tmp2[nm_] = [tl(f"{nm_}{i}", sh_, F32, es) for i in range(2)]
        idx = [tl(f"idx{i}", [128, 2], I32, es) for i in range(2)]
        d_yc = [m.dsem(f"d_yc{i}") for i in range(2)]
        d_xr = [m.dsem(f"d_xr{i}") for i in range(2)]
        d_x1 = [m.dsem(f"d_x1{i}") for i in range(2)]
        d_sc = [m.dsem(f"d_sc{i}") for i in range(2)]
        d_wr = m.dsem("d_wr")
        DMA(sp, d_wr, wr.t[:], I["w_router"].rearrange("(k p) n -> p k n", p=128), writes=[wr])
        DMA(sp, d_wr, br.t[:], I["b_router"], writes=[br])
        YAv = YA_d.rearrange("(a e) p n -> e p a n", e=2)

        def load_ycat(b):
            y = ycat[b % 2]
            cols = slice(b * 512, (b + 1) * 512)
            DMA(sp, d_yc[b % 2], y.t[:, 0:4, :], YC_d[:, :, cols].rearrange("j p n -> p j n"), reads=[B_YC], writes=[y])
            for e in range(2):
                DMA(act, d_yc[b % 2], y.t[e * 64:(e + 1) * 64, 4:8, :], YAv[e][:, :, cols], reads=[B_YA], writes=[y])

        def load_xr(t):
            DMA(sp, d_xr[t % 2], xr[t % 2].t[:], I["x"][t * 128:(t + 1) * 128, :], writes=[xr[t % 2]])

        load_ycat(0)
        load_xr(0)
        for b in range(NB):
            if b + 1 < NB:
                load_ycat(b + 1)
            y = ycat[b % 2]
            for r in range(4):
                t = b * 4 + r
                tok = slice(r * 128, (r + 1) * 128)
                if t + 1 < NT:
                    load_xr(t + 1)
                x = xr[t % 2]
                xo = x1[t % 2]
                s = sm[t % 2]
                junk, hm, h2f, h2T, L, goh, gex, t48, ein, oh1, oh2, msk8, St, Cb, t32 = (tmp2[nm_][t % 2] for nm_ in (
                    "junkb", "hm", "h2f", "h2T", "L", "goh", "gex", "t48", "ein", "oh1", "oh2", "msk8", "St", "Cb", "t32"))
                for hf in range(2):
                    pm = bank("mm", [2, 3, 4, 5])
                    for k in range(8):
                        O(pe, "matmul", reads=[y, wout], writes=[pm], signal=(k == 7), out=pm.t[:], lhsT=y.t[:, k, tok],
                          rhs=wout.t[:, k, hf * 512:(hf + 1) * 512], start=(k == 0), stop=(k == 7))
                    O(dve, "tensor_tensor", reads=[pm, modB], writes=[xo], out=xo.t[:, hf * 512:(hf + 1) * 512], in0=pm.t[:],
                      in1=GT_A[:, hf * 512:(hf + 1) * 512], op=ALU.mult)
                O(pool, "tensor_tensor", reads=[xo, x], writes=[xo], out=xo.t[:], in0=xo.t[:], in1=x.t[:], op=ALU.add)
                DMA(sp, d_x1[t % 2], out_d[t * 128:(t + 1) * 128, :], xo.t[:], reads=[xo], writes=[B_OUT])
                O(act, "activation", reads=[xo], writes=[junk, s], out=junk.t[:], in_=xo.t[:], func=AF.Square, accum_out=s.t[:, 0:1])
                O(act, "activation", reads=[s], writes=[s], out=s.t[:, 1:2], in_=s.t[:, 0:1], func=AF.Sqrt, scale=1.0 / D, bias=EPS)
                O(dve, "reciprocal", reads=[s], writes=[s], out=s.t[:, 2:3], in_=s.t[:, 1:2])
                O(dve, "scalar_tensor_tensor", reads=[xo, s, modB], writes=[hm], out=hm.t[:], in0=xo.t[:], scalar=s.t[:, 2:3],
                  in1=G_F, op0=ALU.mult, op1=ALU.mult)
                O(pool, "tensor_tensor", reads=[hm, modB], writes=[h2f], out=h2f.t[:], in0=hm.t[:], in1=SH_F, op=ALU.add)
                hb2 = h2b[t % 2]
                O(act, "copy", reads=[h2f], writes=[hb2], out=hb2.t[:], in_=h2f.t[:])
                for hf in range(2):
                    pb = bank("tp", [0, 1])
                    pbv = pb.t[:].rearrange("p (k n) -> p k n", k=4)
                    for k in range(4):
                        kk = hf * 4 + k
                        O(pe, "transpose", reads=[h2f, ident_f], writes=[pb], signal=(k == 3), out=pbv[:, k, :],
                          in_=h2f.t[:, kk * 128:(kk + 1) * 128], identity=ident_f.t[:])
                    O(act, "copy", reads=[pb], writes=[h2T], out=h2T.t[:, hf * 4:(hf + 1) * 4, :], in_=pbv)
                pl = bank("stat", [6, 7])
                for k in range(8):
                    O(pe, "matmul", reads=[h2T, wr], writes=[pl], signal=False, out=pl.t[:, 0:36], lhsT=h2T.t[:, k, :], rhs=wr.t[:, k, :],
                      start=(k == 0), stop=False)
                O(pe, "matmul", reads=[ones_f, br], writes=[pl], out=pl.t[:, 0:36], lhsT=ones_f.t[0:1, :], rhs=br.t[0:1, :],
                  start=False, stop=True)
                O(act, "copy", reads=[pl], writes=[L], out=L.t[:], in_=pl.t[:, 0:36])
                O(dve, "tensor_reduce", reads=[L], writes=[s], out=s.t[:, 3:4], in_=L.t[:, 0:4], axis=AX.X, op=ALU.max)
                O(dve, "tensor_scalar", reads=[L, s], writes=[goh], out=goh.t[:], in0=L.t[:, 0:4], scalar1=s.t[:, 3:4], scalar2=None,
                  op0=ALU.is_equal)
                O(dve, "tensor_scalar", reads=[s], writes=[s], out=s.t[:, 4:5], in0=s.t[:, 3:4], scalar1=-1.0, scalar2=None, op0=ALU.mult)
                O(act, "activation", reads=[L, s], writes=[gex, s], out=gex.t[:], in_=L.t[:, 0:4], func=AF.Exp, bias=s.t[:, 4:5],
                  accum_out=s.t[:, 5:6])
                O(dve, "reciprocal", reads=[s], writes=[s], out=s.t[:, 6:7], in_=s.t[:, 5:6])
                O(dve, "tensor_tensor", reads=[L, goh], writes=[t48], out=t48.t[:], in0=L.t[:, 4:36].rearrange("p (g e) -> p g e", g=4),
                  in1=goh.t[:].unsqueeze(2).to_broadcast([128, 4, 8]), op=ALU.mult)
                O(dve, "tensor_reduce", reads=[t48], writes=[ein], out=ein.t[:], in_=t48.t[:].rearrange("p g e -> p e g"), axis=AX.X,
                  op=ALU.add)
                O(dve, "tensor_reduce", reads=[ein], writes=[s], out=s.t[:, 7:8], in_=ein.t[:], axis=AX.X, op=ALU.max)
                O(dve, "tensor_scalar", reads=[ein, s], writes=[oh1], out=oh1.t[:], in0=ein.t[:], scalar1=s.t[:, 7:8], scalar2=None,
                  op0=ALU.is_equal)
                O(dve, "scalar_tensor_tensor", reads=[oh1, ein], writes=[msk8], out=msk8.t[:], in0=oh1.t[:], scalar=-1e30, in1=ein.t[:],
                  op0=ALU.mult, op1=ALU.add)
                O(dve, "tensor_reduce", reads=[msk8], writes=[s], out=s.t[:, 8:9], in_=msk8.t[:], axis=AX.X, op=ALU.max)
                O(dve, "tensor_scalar", reads=[msk8, s], writes=[oh2], out=oh2.t[:], in0=msk8.t[:], scalar1=s.t[:, 8:9], scalar2=None,
                  op0=ALU.is_equal)
                O(dve, "tensor_tensor", reads=[s], writes=[s], out=s.t[:, 9:10], in0=s.t[:, 8:9], in1=s.t[:, 7:8], op=ALU.subtract)
                O(act, "activation", reads=[s], writes=[s], out=s.t[:, 10:11], in_=s.t[:, 9:10], func=AF.Exp)
                O(dve, "tensor_scalar", reads=[s], writes=[s], out=s.t[:, 11:12], in0=s.t[:, 10:11], scalar1=1.0, scalar2=None, op0=ALU.add)
                O(dve, "reciprocal", reads=[s], writes=[s], out=s.t[:, 12:13], in_=s.t[:, 11:12])
                O(dve, "tensor_tensor", reads=[s], writes=[Wt], out=Wt.t[:, t, 0:1], in0=s.t[:, 12:13], in1=s.t[:, 6:7], op=ALU.mult)
                O(dve, "tensor_tensor", reads=[s, Wt], writes=[Wt], out=Wt.t[:, t, 1:2], in0=s.t[:, 6:7], in1=Wt.t[:, t, 0:1], op=ALU.subtract)
                for kk, oh in enumerate((oh1, oh2)):
                    O(dve, "tensor_tensor", reads=[goh, oh], writes=[OH], out=OH.t[:, t, kk, :].rearrange("p (g e) -> p g e", g=4),
                      in0=goh.t[:].unsqueeze(2).to_broadcast([128, 4, 8]), in1=oh.t[:].unsqueeze(1).to_broadcast([128, 4, 8]), op=ALU.mult)
                O(dve, "tensor_tensor", reads=[OH], writes=[St], out=St.t[:], in0=OH.t[:, t, 0, :], in1=OH.t[:, t, 1, :], op=ALU.add)
                pc_ = bank("stat", [6, 7])
                O(pe, "matmul", reads=[ustr, St], writes=[pc_], signal=False, out=pc_.t[:, 0:32], lhsT=ustr.t[:], rhs=St.t[:], start=True, stop=False)
                O(pe, "matmul", reads=[ones_f, Ssum], writes=[pc_], out=pc_.t[:, 0:32], lhsT=ones_f.t[:], rhs=Ssum.t[:], start=False, stop=True)
                O(dve, "tensor_tensor", reads=[pc_, EOFF], writes=[Cb], out=Cb.t[:], in0=pc_.t[:, 0:32], in1=EOFF.t[:], op=ALU.add)
                O(pool, "tensor_tensor", reads=[Ssum, St], writes=[Ssum], out=Ssum.t[:], in0=Ssum.t[:], in1=St.t[:], op=ALU.add)
                for kk in range(2):
                    O(dve, "tensor_tensor", reads=[OH, Cb], writes=[t32], out=t32.t[:], in0=OH.t[:, t, kk, :], in1=Cb.t[:], op=ALU.mult)
                    O(dve, "tensor_reduce", reads=[t32], writes=[PF], out=PF.t[:, t, kk:kk + 1], in_=t32.t[:], axis=AX.X, op=ALU.add)
                ix = idx[t % 2]
                O(dve, "tensor_copy", reads=[PF], writes=[ix], out=ix.t[:], in_=PF.t[:, t, :])
                for kk in range(2):
                    m.dma(pool, d_sc[t % 2], lambda ix=ix, kk=kk, hb2=hb2: pool.h.indirect_dma_start(
                        out=XS_d, out_offset=bass.IndirectOffsetOnAxis(ap=ix.t[:, kk:kk + 1], axis=0), in_=hb2.t[:], in_offset=None),
                        reads=[hb2.b, ix.b], writes=[B_XS], par=True)
        m.barrier()

    if debug == "B2":
        dbg = nc.dram_tensor("dbg", [128, NT, 6], F32, kind="ExternalOutput").ap()
        d_dbg = m.dsem("d_dbg")
        DMA(sp, d_dbg, dbg[:, :, 0:2], Wt.t[:], reads=[Wt], writes=[B_OUT])
        DMA(sp, d_dbg, dbg[:, :, 2:4], PF.t[:], reads=[PF], writes=[B_OUT])
        m.barrier()
        m.es.close()
        return nc

    ADJ = tl("ADJ", [128, 32], F32)
    with ExitStack() as es:
        cnt = tl("cnt", [128, 32], F32, es)
        cnti = tl("cnti", [128, 32], I32, es)
        ntl = tl("ntl", [128, 32], F32, es)
        tbi = tl("tbi", [128, 32], F32, es)
        tb = tl("tb", [128, 32], F32, es)
        jg = tl("jg", [128, NTILE], F32, es)
        cmp3 = tl("cmp3", [128, NTILE, 32], F32, es)
        ej = tl("ej", [128, NTILE], F32, es)
        sj = tl("sj", [128, NTILE], F32, es)
        rowf = tl("rowf", [128, NTILE], F32, es)
        tabi = tl("tabi", [128, 2, NTILE], I32, es)
        pcn = bank("stat", [6, 7])
        O(pe, "matmul", reads=[ones_f, Ssum], writes=[pcn], out=pcn.t[:, 0:32], lhsT=ones_f.t[:], rhs=Ssum.t[:], start=True, stop=True)
        O(dve, "tensor_scalar", reads=[pcn], writes=[cnt], out=cnt.t[:], in0=pcn.t[:, 0:32], scalar1=127.0, scalar2=None, op0=ALU.add)
        O(dve, "tensor_copy", reads=[cnt], writes=[cnti], out=cnti.t[:], in_=cnt.t[:])
        O(dve, "tensor_scalar", reads=[cnti], writes=[cnti], out=cnti.t[:], in0=cnti.t[:], scalar1=7, scalar2=None,
          op0=ALU.arith_shift_right)
        O(dve, "tensor_copy", reads=[cnti], writes=[ntl], out=ntl.t[:], in_=cnti.t[:])
        tb2 = tl("tb2", [128, 32], F32, es)
        src_, dst_ = ntl, tb2
        for sh in (1, 2, 4, 8, 16):
            O(dve, "tensor_tensor", reads=[src_], writes=[dst_], out=dst_.t[:, sh:32], in0=src_.t[:, sh:32], in1=src_.t[:, 0:32 - sh], op=ALU.add)
            O(act, "copy", reads=[src_], writes=[dst_], par=True, out=dst_.t[:, 0:sh], in_=src_.t[:, 0:sh])
            src_, dst_ = dst_, (tbi if dst_ is tb2 else tb2)
        if src_ is not tbi:
            O(dve, "tensor_copy", reads=[src_], writes=[tbi], out=tbi.t[:], in_=src_.t[:])
        O(dve, "tensor_tensor", reads=[tbi, ntl], writes=[tb], out=tb.t[:], in0=tbi.t[:], in1=ntl.t[:], op=ALU.subtract)
        O(dve, "scalar_tensor_tensor", reads=[tb, EOFF], writes=[ADJ], out=ADJ.t[:], in0=tb.t[:], scalar=-128.0, in1=EOFF.t[:],
          op0=ALU.mult, op1=ALU.add)
        jgi = tl("jgi", [128, NTILE], I32, es)
        O(pool, "iota", writes=[jgi], out=jgi.t[:], pattern=[[1, NTILE]], base=0, channel_multiplier=0)
        O(dve, "tensor_copy", reads=[jgi], writes=[jg], out=jg.t[:], in_=jgi.t[:])
        tbi_b = tbi.t[:].unsqueeze(1).to_broadcast([128, NTILE, 32])
        O(dve, "tensor_tensor", reads=[tbi, jg], writes=[cmp3], out=cmp3.t[:], in0=tbi_b,
          in1=jg.t[:].unsqueeze(2).to_broadcast([128, NTILE, 32]), op=ALU.is_le)
        O(dve, "tensor_reduce", reads=[cmp3], writes=[ej], out=ej.t[:], in_=cmp3.t[:], axis=AX.X, op=ALU.add)
        O(dve, "tensor_tensor", reads=[cmp3, tbi], writes=[cmp3], out=cmp3.t[:], in0=cmp3.t[:], in1=tbi_b, op=ALU.mult)
        O(dve, "tensor_reduce", reads=[cmp3], writes=[sj], out=sj.t[:], in_=cmp3.t[:], axis=AX.X, op=ALU.max)
        inv = tl("inv", [128, NTILE], F32, es)
        O(dve, "tensor_scalar", reads=[ej], writes=[inv], out=inv.t[:], in0=ej.t[:], scalar1=31.5, scalar2=float(2 ** 30),
          op0=ALU.is_gt, op1=ALU.mult)
        O(dve, "tensor_scalar", reads=[ej], writes=[ej], out=ej.t[:], in0=ej.t[:], scalar1=31.0, scalar2=None, op0=ALU.min)
        O(dve, "tensor_tensor", reads=[jg, sj], writes=[rowf], out=rowf.t[:], in0=jg.t[:], in1=sj.t[:], op=ALU.subtract)
        O(dve, "tensor_scalar", reads=[rowf], writes=[rowf], out=rowf.t[:], in0=rowf.t[:], scalar1=128.0, scalar2=None, op0=ALU.mult)
        O(dve, "scalar_tensor_tensor", reads=[ej, rowf], writes=[rowf], out=rowf.t[:], in0=ej.t[:], scalar=float(ECAP), in1=rowf.t[:],
          op0=ALU.mult, op1=ALU.add)
        O(dve, "tensor_scalar", reads=[rowf], writes=[rowf], out=rowf.t[:], in0=rowf.t[:], scalar1=float(32 * ECAP - 128), scalar2=0.0,
          op0=ALU.min, op1=ALU.max)
        pidi = tl("pidi", [128, NTILE], I32, es)
        pidf = tl("pidf", [128, NTILE], F32, es)
        O(pool, "iota", writes=[pidi], out=pidi.t[:], pattern=[[0, NTILE]], base=0, channel_multiplier=1)
        O(dve, "tensor_copy", reads=[pidi], writes=[pidf], out=pidf.t[:], in_=pidi.t[:])
        O(dve, "scalar_tensor_tensor", reads=[ej, pidf], writes=[ej], out=ej.t[:], in0=ej.t[:], scalar=128.0, in1=pidf.t[:],
          op0=ALU.mult, op1=ALU.add)
        O(dve, "tensor_tensor", reads=[rowf, pidf], writes=[rowf], out=rowf.t[:], in0=rowf.t[:], in1=pidf.t[:], op=ALU.add)
        O(dve, "tensor_tensor", reads=[ej, inv], writes=[ej], out=ej.t[:], in0=ej.t[:], in1=inv.t[:], op=ALU.add)
        O(dve, "tensor_tensor", reads=[rowf, inv], writes=[rowf], out=rowf.t[:], in0=rowf.t[:], in1=inv.t[:], op=ALU.add)
        O(dve, "tensor_copy", reads=[ej], writes=[tabi], out=tabi.t[:, 0, :], in_=ej.t[:])
        O(dve, "tensor_copy", reads=[rowf], writes=[tabi], out=tabi.t[:, 1, :], in_=rowf.t[:])
        m.barrier()

        regW = nc.alloc_register(mybir.EngineType.Pool, "bndW")
        regX = nc.alloc_register(mybir.EngineType.Pool, "bndX")
        nc.gpsimd.reg_mov(regW, 32 * 128 - 1)
        nc.gpsimd.reg_mov(regX, 32 * ECAP - 1)
        NBUF = 4
        NW = 3
        xg = [tl(f"xg{i}", [128, D], BF16, es) for i in range(NBUF)]
        wall = [tl(f"wall{i}", [128, 6144], BF16, es) for i in range(NBUF)]
        wg = [Tl(None) for _ in range(NBUF)]
        wu = [Tl(None) for _ in range(NBUF)]
        wd = [Tl(None) for _ in range(NBUF)]
        for i_ in range(NBUF):
            wg[i_].t = wall[i_].t[:, 0:2048].rearrange("p (k f) -> p k f", k=8)
            wu[i_].t = wall[i_].t[:, 2048:4096].rearrange("p (k f) -> p k f", k=8)
            wd[i_].t = wall[i_].t[:, 4096:6144].rearrange("p (j n) -> p j n", j=2)
            wg[i_].b = wu[i_].b = wd[i_].b = wall[i_].b
        xT = [tl(f"xT{i}", [128, 8, 128], BF16, es) for i in range(NW)]
        sgl = [tl(f"sgl{i}", [128, 2, 128], F32, es) for i in range(NW)]
        aT = [tl(f"aT{i}", [128, 2, 128], BF16, es) for i in range(NW)]
        ysb = [tl(f"ysb{i}", [128, D], F32, es) for i in range(NW)]
        d_mx = [m.dsem(f"d_mx{i}") for i in range(NBUF)]
        d_mw = [m.dsem(f"d_mw{i}") for i in range(NBUF)]
        d_ys = [m.dsem(f"d_ys{i}") for i in range(NW)]
        def load_tile(j):
            i = j % NBUF
            m.dma(pool, d_mw[i], lambda: pool.h.indirect_dma_start(
                out=wall[i].t[:], out_offset=None, in_=WA_d, in_offset=bass.IndirectOffsetOnAxis(ap=tabi.t[:, 0, j:j + 1], axis=0),
                bounds_check=regW, oob_is_err=False),
                reads=[B_WE, tabi.b], writes=[wall[i].b])
            m.dma(pool, d_mx[i], lambda: pool.h.indirect_dma_start(
                out=xg[i].t[:], out_offset=None, in_=XS_d, in_offset=bass.IndirectOffsetOnAxis(ap=tabi.t[:, 1, j:j + 1], axis=0),
                bounds_check=regX, oob_is_err=False),
                reads=[B_XS, tabi.b], writes=[xg[i].b])

        for j in range(min(NBUF - 1, NTILE)):
            load_tile(j)
        for j in range(NTILE):
            if j + NBUF - 1 < NTILE:
                load_tile(j + NBUF - 1)
            i = j % NBUF
            pb = bank("tp", [0, 1])
            pbv = pb.t[:].bitcast(BF16).rearrange("p (k n) -> p k n", k=8)
            for k in range(8):
                O(pe, "transpose", reads=[xg[i], ident_b], writes=[pb], signal=(k == 7), out=pbv[:, k, :], in_=xg[i].t[:, k::8],
                  identity=ident_b.t[:])
            xt_ = xT[j % NW]
            O(dve, "tensor_copy", reads=[pb], writes=[xt_], out=xt_.t[:], in_=pbv)
            pgu = bank("mmoe", [2, 3, 4, 5, 6, 7])
            pguv = pgu.t[:].rearrange("p (c n) -> p c n", c=4)
            for c in range(4):
                w_ = wg[i] if c < 2 else wu[i]
                fc = c % 2
                for k in range(8):
                    O(pe, "matmul", reads=[w_, xt_], writes=[pgu], signal=(c == 3 and k == 7), out=pguv[:, c, :],
                      lhsT=w_.t[:, k, fc * 128:(fc + 1) * 128], rhs=xt_.t[:, k, :], start=(k == 0), stop=(k == 7))
            sg_ = sgl[j % NW]
            a_ = aT[j % NW]
            O(act, "activation", reads=[pgu], writes=[sg_], out=sg_.t[:], in_=pguv[:, 0:2, :], func=AF.Silu)
            O(dve, "tensor_tensor", reads=[pgu, sg_], writes=[a_], out=a_.t[:], in0=pguv[:, 2:4, :], in1=sg_.t[:], op=ALU.mult)
            ys_ = ysb[j % NW]
            for hf in range(2):
                py = bank("mmoe", [2, 3, 4, 5, 6, 7])
                for jc in range(2):
                    O(pe, "matmul", reads=[a_, wd[i]], writes=[py], signal=(jc == 1), out=py.t[:], lhsT=a_.t[:, jc, :],
                      rhs=wd[i].t[:, jc, hf * 512:(hf + 1) * 512], start=(jc == 0), stop=(jc == 1))
                if hf == 0:
                    O(act, "copy", reads=[py], writes=[ys_], out=ys_.t[:, 0:512], in_=py.t[:])
                else:
                    O(dve, "tensor_copy", reads=[py], writes=[ys_], out=ys_.t[:, 512:1024], in_=py.t[:])
            DMA(act, d_ys[j % NW], YS_d[j * 128:(j + 1) * 128, :], ys_.t[:], reads=[ys_], writes=[B_YS])
        m.barrier()

    with ExitStack() as es:
        NF = 4
        y0 = [tl(f"y0{i}", [128, D], F32, es) for i in range(NF)]
        y1 = [tl(f"y1{i}", [128, D], F32, es) for i in range(NF)]
        xf = [tl(f"xf{i}", [128, D], F32, es) for i in range(NF)]
        acc = [tl(f"acc{i}", [128, D], F32, es) for i in range(NF)]
        t32b = tl("t32b", [128, 32], F32, es)
        adjs = tl("adjs", [128, NT, 2], F32, es)
        yidx = tl("yidx", [128, NT, 2], I32, es)
        d_g0 = [m.dsem(f"d_g0{i}") for i in range(NF)]
        d_g1 = [m.dsem(f"d_g1{i}") for i in range(NF)]
        d_xf = [m.dsem(f"d_xf{i}") for i in range(NF)]
        d_of = [m.dsem(f"d_of{i}") for i in range(NF)]
        for t in range(NT):
            for kk in range(2):
                O(dve, "tensor_tensor", reads=[OH, ADJ], writes=[t32b], out=t32b.t[:], in0=OH.t[:, t, kk, :], in1=ADJ.t[:], op=ALU.mult)
                O(dve, "tensor_reduce", reads=[t32b], writes=[adjs], out=adjs.t[:, t, kk:kk + 1], in_=t32b.t[:], axis=AX.X, op=ALU.add)
        O(dve, "tensor_tensor", reads=[PF, adjs], writes=[adjs], out=adjs.t[:], in0=PF.t[:], in1=adjs.t[:], op=ALU.subtract)
        O(dve, "tensor_scalar", reads=[adjs], writes=[adjs], out=adjs.t[:], in0=adjs.t[:], scalar1=float(NTILE * 128 - 1), scalar2=0.0,
          op0=ALU.min, op1=ALU.max)
        O(dve, "tensor_copy", reads=[adjs], writes=[yidx], out=yidx.t[:], in_=adjs.t[:])

        def load_fin(t):
            i = t % NF
            m.dma(pool, d_g0[i], lambda: pool.h.indirect_dma_start(
                out=y0[i].t[:], out_offset=None, in_=YS_d, in_offset=bass.IndirectOffsetOnAxis(ap=yidx.t[:, t, 0:1], axis=0)),
                reads=[B_YS, yidx.b], writes=[y0[i].b])
            m.dma(pool, d_g1[i], lambda: pool.h.indirect_dma_start(
                out=y1[i].t[:], out_offset=None, in_=YS_d, in_offset=bass.IndirectOffsetOnAxis(ap=yidx.t[:, t, 1:2], axis=0)),
                reads=[B_YS, yidx.b], writes=[y1[i].b])
            DMA(act, d_xf[i], xf[i].t[:], out_d[t * 128:(t + 1) * 128, :], reads=[B_OUT], writes=[xf[i]])

        for t in range(NF - 1):
            load_fin(t)
        B_OUT2 = Buf()
        for t in range(NT):
            if t + NF - 1 < NT:
                load_fin(t + NF - 1)
            i = t % NF
            a = acc[i]
            O(act, "activation", reads=[y0[i], Wt], writes=[a], out=a.t[:], in_=y0[i].t[:], func=AF.Identity, scale=Wt.t[:, t, 0:1])
            O(dve, "scalar_tensor_tensor", reads=[y1[i], Wt, a], writes=[a], out=a.t[:], in0=y1[i].t[:], scalar=Wt.t[:, t, 1:2],
              in1=a.t[:], op0=ALU.mult, op1=ALU.add)
            O(dve, "tensor_tensor", reads=[a, modB], writes=[a], out=a.t[:], in0=a.t[:], in1=GT_F, op=ALU.mult)
            O(dve if t % 2 == 0 else pool, "tensor_tensor", reads=[a, xf[i]], writes=[a], out=a.t[:], in0=a.t[:], in1=xf[i].t[:], op=ALU.add)
            DMA(sp, d_of[i], out_d[t * 128:(t + 1) * 128, :], a.t[:], reads=[a, xf[i]], writes=[B_OUT2])
        m.barrier()
    m.es.close()
    return nc


def prep_inputs(inputs):
    f = lambda a: np.ascontiguousarray(np.asarray(a))
    g = lambda name: np.asarray(inputs[name])[0]
    shared = {
        "w_ada": f(g("w_ada")),
        "b_ada": f(g("b_ada").reshape(1, -1)),
        "norm_mix": f(g("norm_mix").reshape(1, -1)),
        "w_in": f(g("w_in")),
        "conv_w_c": f(g("conv_w").reshape(31, 4, 128).transpose(2, 1, 0)),
        "conv_b_c": f(g("conv_b").reshape(4, 128).T),
        "conv_ln_g_c": f(g("conv_ln_g").reshape(4, 128).T),
        "conv_ln_b_c": f(g("conv_ln_b").reshape(4, 128).T),
        "q_a_norm_c": f(g("q_a_norm").reshape(2, 128).T),
        "w_q_b": f(g("w_q_b")),
        "kv_a_norm_c": f(g("kv_a_norm").reshape(1, 128).T),
        "w_kv_b": f(g("w_kv_b")),
        "q_norm": f(g("q_norm").reshape(1, -1)),
        "k_norm": f(g("k_norm").reshape(1, -1)),
        "w_out": f(g("w_out")),
        "norm_ffn": f(g("norm_ffn").reshape(1, -1)),
        "w_router": f(np.concatenate([g("w_group"), g("w_expert")], axis=1)),
        "b_router": f(np.concatenate([g("b_group"), g("b_expert")]).reshape(1, -1)),
        "w_gate_e": f(g("w_gate_e")),
        "w_up_e": f(g("w_up_e")),
        "w_down_e": f(g("w_down_e")),
    }
    x = np.asarray(inputs["x"])
    c = np.asarray(inputs["c"])
    pos = np.asarray(inputs["positions"])
    maps = []
    for b in range(8):
        d = dict(shared)
        d["x"] = f(x[b])
        d["c_pk"] = f(c[b].reshape(8, 128).T)
        d["pos_pj"] = f(pos[b].reshape(NT, 128).T.astype(np.int32))
        maps.append(d)
    return maps


_NC = None


def kernel(**inputs):
    global _NC
    if _NC is None:
        _NC = build()
    maps = prep_inputs(inputs)
    res = run_bass_kernel_spmd(_NC, maps, core_ids=list(range(8)))
    return np.stack([np.asarray(r["out"]) for r in res.results], axis=0).astype(np.float32)
```

```python
import math
from contextlib import ExitStack

import numpy as np
import concourse.bass as bass
import concourse.mybir as mybir
from concourse.bass_utils import run_bass_kernel_spmd

F32 = mybir.dt.float32
BF16 = mybir.dt.bfloat16
I32 = mybir.dt.int32
ALU = mybir.AluOpType
AF = mybir.ActivationFunctionType
AX = mybir.AxisListType

S = 4096
D = 1024
NT = 32
NB = 8
EPS = 1e-6
import os
STAGE = int(os.environ.get('MK_STAGE', '9'))
INORDER = bool(int(os.environ.get('MK_INORDER', '0')))
NTILE = 96
ECAP = 4096


class Stream:
    def __init__(self, sem, step, name):
        self.sem, self.step, self.count, self.name = sem, step, 0, name
        self.nobarrier = False


class Eng(Stream):
    def __init__(self, handle, sem, name, self_sync=True, speed=1000.0, fixed=0.08):
        super().__init__(sem, 1, name)
        self.h = handle
        self.seen = {}
        self.self_sync = self_sync
        self.speed = speed
        self.fixed = fixed
        self.free_t = 0.0


class Buf:
    __slots__ = ("w", "r", "name", "ps")

    def __init__(self, name="", ps=False):
        self.w, self.r, self.name, self.ps = set(), set(), name, ps


class Tl:
    def __init__(self, t, name=""):
        self.t = t
        self.b = Buf(name)


class Op:
    __slots__ = ("eng", "fns", "deps", "dur", "lat", "ds", "idx", "ticket", "stream", "done_t", "nsucc", "succ", "npend", "prio")

    def __init__(self, eng, fns, dur, ds=None, lat=0.0):
        self.eng, self.fns, self.dur, self.ds, self.lat = eng, fns, dur, ds, lat
        self.deps = set()
        self.ticket = None
        self.stream = None
        self.done_t = 0.0
        self.succ = []
        self.npend = 0
        self.prio = None


def _ap_elems(kw):
    ap = kw.get("out", None)
    if ap is None:
        ap = kw.get("ap", None)
    try:
        sh = ap.shape
        n = 1
        for d in sh[1:]:
            n *= d
        return n
    except Exception:
        return 512


class MK:
    def __init__(self, nc):
        self.nc = nc
        self.es = ExitStack()
        self.dsems = []
        self.pe = Eng(nc.tensor, self.sem("pe"), "pe", self_sync=False, speed=2400.0, fixed=0.03)
        self.act = Eng(nc.scalar, self.sem("act"), "act", speed=1000.0, fixed=0.2)
        self.dve = Eng(nc.vector, self.sem("dve"), "dve", speed=900.0, fixed=0.16)
        self.pool = Eng(nc.gpsimd, self.sem("pool"), "pool", speed=420.0, fixed=0.15)
        self.sp = Eng(nc.sync, self.sem("sp"), "sp")
        self.engs = [self.pe, self.act, self.dve, self.pool, self.sp]
        if int(os.environ.get("MK_NOSELF", "0")):
            for e_ in self.engs:
                e_.self_sync = False
        self.pending = []
        self.grp = None
        self.seg = None

    def begin_seg(self, slot):
        assert self.grp is None
        self.seg = (float(slot), [])

    def end_seg(self):
        assert self.grp is None
        slot, ops = self.seg
        n = max(len(ops), 1)
        if int(os.environ.get("MK_NOSEG", "0")):
            ops = []
        for k, o in enumerate(ops):
            o.prio = slot + (k + 0.5) / n
        self.seg = None

    def sem(self, name):
        return self.es.enter_context(self.nc.semaphore(name))

    def dsem(self, name):
        d = Stream(self.sem(name), 16, name)
        self.dsems.append(d)
        return d

    def sb(self, name, shape, dt, es=None):
        return (es or self.es).enter_context(self.nc.sbuf_tensor(name, shape, dt))

    def ps(self, name, shape, dt):
        return self.es.enter_context(self.nc.psum_tensor(name, shape, dt))

    def _record(self, op, reads, writes, par):
        for b in reads:
            op.deps |= b.w
            if b.ps:
                op.deps |= {r_ for r_ in b.r if r_.eng is not op.eng}
        for b in writes:
            if not par:
                op.deps |= b.w
            op.deps |= b.r
        op.deps.discard(op)
        for b in reads:
            b.r.add(op)
        for b in writes:
            if par:
                b.w = set(b.w)
                b.w.add(op)
            else:
                b.w = {op}
            b.r = set()

    def op(self, eng, fn, reads=(), writes=(), signal=True, dur=None, par=False):
        if dur is None:
            dur = eng.fixed + 512 / eng.speed
        if eng is self.pe:
            if self.grp is None:
                self.grp = (Op(eng, [], 0.0), set(), set())
            g, gr, gw = self.grp
            g.fns.append(fn)
            g.dur += dur
            gr.update(reads)
            gw.update(writes)
            if signal:
                self.grp = None
                self.pending.append(g)
                if self.seg is not None:
                    self.seg[1].append(g)
                self._record(g, gr, gw, par)
            return g
        o = Op(eng, [fn], dur)
        self.pending.append(o)
        if self.seg is not None:
            self.seg[1].append(o)
        self._record(o, reads, writes, par)
        return o

    def dma(self, eng, ds, fn, reads=(), writes=(), nbytes=1 << 20, par=False, lat=None):
        o = Op(eng, [fn], 0.15 if eng is not self.pool else 1.0, ds=ds, lat=(2.0 + nbytes / 150e3) if lat is None else lat)
        self.pending.append(o)
        if self.seg is not None:
            self.seg[1].append(o)
        self._record(o, reads, writes, par)
        return o

    def flush(self):
        assert self.grp is None
        ops = self.pending
        self.pending = []
        pend_set = set(ops)
        for i, o in enumerate(ops):
            o.idx = i
            o.deps = {d for d in o.deps if d in pend_set or d.ticket is not None}
        keep = set(os.environ.get("MK_KEEP", "").split(","))
        last_on = {}
        for o in ops:
            o.npend = 0
            for d in o.deps:
                if d in pend_set:
                    d.succ.append(o)
                    o.npend += 1
            if o.eng.name in keep:
                p = last_on.get(o.eng)
                if p is not None and p not in o.deps:
                    p.succ.append(o)
                    o.npend += 1
                last_on[o.eng] = o
        ready = [o for o in ops if o.npend == 0]
        t_base = max(e.free_t for e in self.engs)
        for e in self.engs:
            e.free_t = t_base
        nleft = len(ops)
        while nleft:
            best, bt = None, None
            for o in ready:
                st = o.eng.free_t
                for d in o.deps:
                    dt_ = d.done_t + (0.90 if d.eng is not o.eng else 0.08)
                    if dt_ > st:
                        st = dt_
                key = (st, o.idx) if not INORDER else ((o.prio if o.prio is not None else -1.0), o.idx)
                if bt is None or key < bt:
                    best, bt = o, key
            o = best
            ready.remove(o)
            st = bt[0]
            o.eng.free_t = st + o.dur
            o.done_t = st + o.dur + o.lat
            self._emit(o)
            nleft -= 1
            for s_ in o.succ:
                s_.npend -= 1
                if s_.npend == 0:
                    ready.append(s_)
            o.succ = []

    def _emit(self, o):
        eng = o.eng
        need = {}
        for d in o.deps:
            s, t = d.stream, d.ticket
            if need.get(s, 0) < t:
                need[s] = t
        for s, t in need.items():
            if s is eng and not eng.self_sync:
                continue
            if eng.seen.get(s, 0) >= t:
                continue
            eng.h.wait_ge(s.sem, t * s.step)
            eng.seen[s] = t
        inst = None
        for fn in o.fns:
            inst = fn()
        if o.ds is not None:
            o.ds.count += 1
            inst.then_inc(o.ds.sem, 16)
            o.stream, o.ticket = o.ds, o.ds.count
        else:
            eng.count += 1
            inst.then_inc(eng.sem, 1)
            o.stream, o.ticket = eng, eng.count
        o.deps = set()

    def barrier(self):
        self.flush()
        streams = list(self.engs) + [d for d in self.dsems if not getattr(d, "nobarrier", False)]
        for e in self.engs:
            for s in streams:
                if s.count == 0 or e.seen.get(s, 0) >= s.count:
                    continue
                e.h.wait_ge(s.sem, s.count * s.step)
                e.seen[s] = s.count


def declare_inputs(nc):
    I = {}

    def inp(name, shape, dt=F32):
        I[name] = nc.dram_tensor(name, shape, dt, kind="ExternalInput").ap()

    inp("x", [S, D])
    inp("c_pk", [128, 8])
    inp("pos_pj", [128, NT], I32)
    inp("w_ada", [D, 6 * D])
    inp("b_ada", [1, 6 * D])
    inp("norm_mix", [1, D])
    inp("w_in", [D, 1440])
    inp("conv_w_c", [128, 4, 31])
    inp("conv_b_c", [128, 4])
    inp("conv_ln_g_c", [128, 4])
    inp("conv_ln_b_c", [128, 4])
    inp("q_a_norm_c", [128, 2])
    inp("w_q_b", [256, 768])
    inp("kv_a_norm_c", [128, 1])
    inp("w_kv_b", [128, 1024])
    inp("q_norm", [1, 96])
    inp("k_norm", [1, 96])
    inp("w_out", [D, D])
    inp("norm_ffn", [1, D])
    inp("w_router", [D, 36])
    inp("b_router", [1, 36])
    inp("w_gate_e", [32, D, 256])
    inp("w_up_e", [32, D, 256])
    inp("w_down_e", [32, 256, D])
    return I


class _Stop(Exception):
    pass


def build(debug=None):
    holder = {}
    try:
        _build(debug, holder)
    except _Stop:
        holder["m"].es.close()
    return holder["nc"]


def _build(debug, holder):
    nc = bass.Bass("TRN2", target_bir_lowering=False)
    I = declare_inputs(nc)
    out_d = nc.dram_tensor("out", [S, D], F32, kind="ExternalOutput").ap()
    m = MK(nc)
    holder["nc"], holder["m"] = nc, m
    pe, act, dve, pool, sp = m.pe, m.act, m.dve, m.pool, m.sp

    def scr(name, shape, dt, dbg=False):
        kind = "ExternalOutput" if dbg else "Internal"
        return nc.dram_tensor(name, shape, dt, kind=kind).ap()

    dA = (debug or "").startswith("A")
    QT_d = scr("QT_d", [8, 96, S], BF16, dA)
    KT_d = scr("KT_d", [8, 96, S], BF16, dA)
    V_d = scr("V_d", [8, 128, NT, 64], BF16, dA)
    YC_d = scr("YC_d", [4, 128, S], BF16, dA)
    YA_d = scr("YA_d", [8, 64, S], BF16, debug == "B1")
    XS_d = scr("XS_d", [32 * ECAP, D], BF16)
    YS_d = scr("YS_d", [NTILE * 128, D], F32)
    WA_d = scr("WA_d", [32 * 128, 6144], BF16)
    WA3 = WA_d.rearrange("(e p) n -> e p n", p=128)
    B_QT, B_KT, B_V, B_YC, B_YA, B_XS, B_YS, B_WE, B_OUT = (Buf() for _ in range(9))

    psb = [Tl(m.ps(f"psb{i}", [128, 512], F32), f"psb{i}") for i in range(8)]
    for p_ in psb:
        p_.b.ps = True
    rr = {}

    def bank(role, banks):
        i = rr.get(role, 0)
        rr[role] = i + 1
        return psb[banks[i % len(banks)]]

    def tl(name, shape, dt, es=None):
        return Tl(m.sb(name, shape, dt, es), name)

    def O(eng, fname, reads=(), writes=(), signal=True, par=False, **kw):
        n = _ap_elems(kw)
        if eng is pe:
            mult = 4.0 if (kw.get("lhsT", kw.get("in_")).dtype == F32) else 1.0
            dur = eng.fixed + mult * max(n, 64) / eng.speed
        else:
            dur = eng.fixed + n / eng.speed
        return m.op(eng, lambda: getattr(eng.h, fname)(**kw), [r.b if isinstance(r, Tl) else r for r in reads],
                    [w.b if isinstance(w, Tl) else w for w in writes], signal, dur=dur, par=par)

    def DMA(eng, ds, out, in_, reads=(), writes=(), lat=None):
        return m.dma(eng, ds, lambda: eng.h.dma_start(out=out, in_=in_),
                     [r.b if isinstance(r, Tl) else r for r in reads],
                     [w.b if isinstance(w, Tl) else w for w in writes], lat=lat)

    ident_f = tl("ident_f", [128, 128], F32)
    ident_b = tl("ident_b", [128, 128], BF16)
    ones_f = tl("ones_f", [128, 128], F32)
    ones_b = tl("ones_b", [128, 128], BF16)

    negM = tl("negM", [128, 8], F32)
    O(pool, "memset", writes=[ident_f], ap=ident_f.t[:], constant=0.0)
    O(pool, "affine_select", reads=[ident_f], writes=[ident_f], out=ident_f.t[:], in_=ident_f.t[:],
      pattern=[[-1, 128]], compare_op=ALU.not_equal, fill=1.0, base=0, channel_multiplier=1)
    O(pool, "tensor_copy", reads=[ident_f], writes=[ident_b], out=ident_b.t[:], in_=ident_f.t[:])
    O(pool, "memset", writes=[ones_f], ap=ones_f.t[:], constant=1.0)
    O(pool, "memset", writes=[ones_b], ap=ones_b.t[:], constant=1.0)

    d_ld = [m.dsem(f"d_ld{i}") for i in range(4)]
    d_misc = m.dsem("d_misc")
    d_nrm = m.dsem("d_nrm")

    def adaln_parts(tag, es, banks, evac):
        d_c1, d_c2 = m.dsem("d_c1" + tag), m.dsem("d_c2" + tag)
        d_nrm_t = m.dsem("d_nrmA" + tag)
        c_sb = tl("c_sb" + tag, [128, 8], F32, es)
        cs = tl("cs" + tag, [128, 8], F32, es)
        csb = tl("csb" + tag, [128, 8, 128], F32, es)
        bada = tl("bada" + tag, [1, 6 * D], F32, es)
        wst = [tl(f"wst{i}" + tag, [128, 8, 512], F32, es) for i in range(2)]
        wsth = []
        for i in range(2):
            hv = []
            for hh in range(2):
                v = Tl(None, f"wst{i}h{hh}" + tag)
                v.t = wst[i].t[:, hh * 4:(hh + 1) * 4, :]
                hv.append(v)
            wsth.append(hv)
        nrm = tl("nrm" + tag, [128, D], F32, es)
        DMA(sp, d_c1, c_sb.t[:], I["c_pk"], writes=[c_sb])
        DMA(sp, d_c2, bada.t[:], I["b_ada"], writes=[bada])
        O(act, "activation", reads=[c_sb], writes=[cs], out=cs.t[:], in_=c_sb.t[:], func=AF.Silu)
        for k in range(8):
            O(dve, "tensor_scalar", reads=[cs, ones_f], writes=[csb], out=csb.t[:, k, :], in0=ones_f.t[:],
              scalar1=cs.t[:, k:k + 1], scalar2=None, op0=ALU.mult)
        wv = I["w_ada"].rearrange("(k p) n -> p k n", p=128)

        def chunk(j, dst, base, gate=(), wlat=None):
            wh = wsth[j % 2]
            for hh in range(2):
                DMA(sp if hh == 0 else act, d_ld[(j % 2) * 2 + hh], wh[hh].t,
                    wv[:, hh * 4:(hh + 1) * 4, j * 512:(j + 1) * 512], reads=gate, writes=[wh[hh]], lat=wlat)
            pb = bank("mm" + tag, banks)
            for k in range(8):
                O(pe, "matmul", reads=[csb, wh[k // 4]], writes=[pb], signal=False, out=pb.t[:], lhsT=csb.t[:, k, :],
                  rhs=wh[k // 4].t[:, k % 4, :], start=(k == 0), stop=False)
            O(pe, "matmul", reads=[ones_f, bada], writes=[pb], out=pb.t[:], lhsT=ones_f.t[0:1, :],
              rhs=bada.t[0:1, j * 512:(j + 1) * 512], start=False, stop=True)
            if evac is act:
                O(act, "copy", reads=[pb], writes=[dst], out=dst.t[:, (j - base) * 512:(j - base + 1) * 512], in_=pb.t[:])
            else:
                O(dve, "tensor_copy", reads=[pb], writes=[dst], out=dst.t[:, (j - base) * 512:(j - base + 1) * 512], in_=pb.t[:])

        def finish(dst, gsecs):
            for (gsec, nm) in gsecs:
                DMA(sp, d_nrm_t, nrm.t[:], I[nm].partition_broadcast(128), writes=[nrm])
                O(dve, "scalar_tensor_tensor", reads=[dst, nrm], writes=[dst], out=gsec, in0=gsec, scalar=1.0,
                  in1=nrm.t[:], op0=ALU.add, op1=ALU.mult)

        return chunk, finish

    def adaln(chunks, dst, base, gsecs, tag=""):
        with ExitStack() as es_own:
            chunk, finish = adaln_parts(tag, es_own, [2, 3], act)
            for j in chunks:
                chunk(j, dst, base)
            finish(dst, gsecs)
            m.barrier()

    if debug == "S0":
        m.es.close()
        return nc
    d_pc = m.dsem("d_pc")
    d_pc.nobarrier = True

    def precast(e, gate=()):
        m.dma(pool, d_pc, lambda: pool.h.dma_start(out=WA3[e][:, 0:2048], in_=I["w_gate_e"][e].rearrange("(p k) f -> p (k f)", k=8)),
              reads=gate, writes=[B_WE], par=True)
        m.dma(pool, d_pc, lambda: pool.h.dma_start(out=WA3[e][:, 2048:4096], in_=I["w_up_e"][e].rearrange("(p k) f -> p (k f)", k=8)),
              reads=gate, writes=[B_WE], par=True)
        m.dma(pool, d_pc, lambda: pool.h.dma_start(out=WA3[e][:, 4096:6144].rearrange("p (j n) -> p j n", j=2),
                                                   in_=I["w_down_e"][e].rearrange("(j p) n -> p j n", p=128)),
              reads=gate, writes=[B_WE], par=True)

    with ExitStack() as es:
        modA = tl("modA", [128, 2 * D], F32, es)
        SH_A, G_A = modA.t[:, 0:D], modA.t[:, D:2 * D]
        win = tl("win", [128, 8, 1440], BF16, es)
        wqb = tl("wqb", [128, 2, 768], BF16, es)
        wkvb = tl("wkvb", [128, 1024], BF16, es)
        diag = tl("diag", [128, 4, 31, 128], BF16, es)
        cw = tl("cw", [128, 4, 31], F32, es)
        cb = tl("cb", [128, 4], F32, es)
        lng = tl("lng", [128, 4], F32, es)
        lnb = tl("lnb", [128, 4], F32, es)
        gq = tl("gq", [128, 8, 96], F32, es)
        gk = tl("gk", [128, 8, 96], F32, es)
        cos_t = tl("cos_t", [128, NT, 16], F32, es)
        sin_t = tl("sin_t", [128, NT, 16], F32, es)
        avg_f = tl("avg_f", [128, 128], F32, es)
        d_w = m.dsem("d_w")
        m.dma(pool, d_w, lambda: pool.h.dma_start(out=win.t[:], in_=I["w_in"].rearrange("(k p) n -> p k n", p=128)),
              writes=[win.b])
        with ExitStack() as es2:
            wq_st = tl("wq_st", [128, 2, 768], F32, es2)
            wkv_st = tl("wkv_st", [128, 1024], F32, es2)
            qan = tl("qan", [128, 2], F32, es2)
            kvan = tl("kvan", [128, 1], F32, es2)
            gtmp = tl("gtmp", [128, 96], F32, es2)
            posi = tl("posi", [128, NT], I32, es2)
            posf = tl("posf", [128, NT], F32, es2)
            ang = tl("ang", [128, NT, 16], F32, es2)
            rk = tl("rk", [128, NT, 16], F32, es2)
            rki = tl("rki", [128, NT, 16], I32, es2)
            rr_ = tl("rr_", [128, NT, 16], F32, es2)
            msk = tl("msk", [128, NT, 16], F32, es2)
            dm = [m.dsem(f"d_misc{i}") for i in range(9)]
            DMA(sp, dm[0], wq_st.t[:], I["w_q_b"].rearrange("(k p) n -> p k n", p=128), writes=[wq_st])
            DMA(sp, dm[1], wkv_st.t[:], I["w_kv_b"], writes=[wkv_st])
            DMA(sp, dm[2], qan.t[:], I["q_a_norm_c"], writes=[qan])
            DMA(sp, dm[3], kvan.t[:], I["kv_a_norm_c"], writes=[kvan])
            DMA(sp, dm[4], cw.t[:], I["conv_w_c"], writes=[cw])
            DMA(sp, dm[5], cb.t[:], I["conv_b_c"], writes=[cb])
            DMA(sp, dm[6], lng.t[:], I["conv_ln_g_c"], writes=[lng])
            DMA(sp, dm[7], lnb.t[:], I["conv_ln_b_c"], writes=[lnb])
            DMA(sp, dm[8], posi.t[:], I["pos_pj"], writes=[posi])
            for k in range(2):
                O(dve, "tensor_scalar", reads=[wq_st, qan], writes=[wqb], out=wqb.t[:, k, :], in0=wq_st.t[:, k, :],
                  scalar1=qan.t[:, k:k + 1], scalar2=None, op0=ALU.mult)
            O(dve, "tensor_scalar", reads=[wkv_st, kvan], writes=[wkvb], out=wkvb.t[:], in0=wkv_st.t[:],
              scalar1=kvan.t[:, 0:1], scalar2=None, op0=ALU.mult)
            for (g, nm, sc) in ((gq, "q_norm", 96.0 ** -0.5), (gk, "k_norm", 1.0)):
                DMA(sp, d_nrm, gtmp.t[:], I[nm].partition_broadcast(128), writes=[gtmp])
                for h in range(8):
                    O(dve, "tensor_scalar", reads=[gtmp], writes=[g], out=g.t[:, h, :], in0=gtmp.t[:], scalar1=sc,
                      scalar2=None, op0=ALU.mult)
            for ci, g in enumerate((gq, gk)):
                c0 = 4 + 2 * ci
                O(dve, "tensor_reduce", reads=[g], writes=[negM], out=negM.t[:, c0:c0 + 1], in_=g.t[:, 0, :], axis=AX.X, op=ALU.max)
                O(dve, "tensor_reduce", reads=[g], writes=[negM], out=negM.t[:, c0 + 1:c0 + 2], in_=g.t[:, 0, :], axis=AX.X, op=ALU.min)
                O(dve, "tensor_scalar", reads=[negM], writes=[negM], out=negM.t[:, c0 + 1:c0 + 2], in0=negM.t[:, c0 + 1:c0 + 2], scalar1=-1.0,
                  scalar2=None, op0=ALU.mult)
                O(dve, "tensor_tensor", reads=[negM], writes=[negM], out=negM.t[:, 1 + ci:2 + ci], in0=negM.t[:, c0:c0 + 1],
                  in1=negM.t[:, c0 + 1:c0 + 2], op=ALU.max)
            O(dve, "tensor_tensor", reads=[negM], writes=[negM], out=negM.t[:, 3:4], in0=negM.t[:, 1:2], in1=negM.t[:, 2:3], op=ALU.mult)
            O(dve, "tensor_scalar", reads=[negM], writes=[negM], out=negM.t[:, 0:1], in0=negM.t[:, 3:4], scalar1=-96.0, scalar2=None, op0=ALU.mult)
            for j in range(4):
                for k in range(31):
                    if (j * 31 + k) % 2 == 0:
                        O(dve, "tensor_scalar", reads=[ident_f, cw], writes=[diag], par=True, out=diag.t[:, j, k, :], in0=ident_f.t[:],
                          scalar1=cw.t[:, j, k:k + 1], scalar2=None, op0=ALU.mult)
                    else:
                        O(act, "activation", reads=[ident_f, cw], writes=[diag], par=True, out=diag.t[:, j, k, :], in_=ident_f.t[:],
                          func=AF.Identity, scale=cw.t[:, j, k:k + 1])
            O(pool, "memset", writes=[avg_f], ap=avg_f.t[:], constant=1.0 / 512.0)
            O(dve, "tensor_copy", reads=[posi], writes=[posf], out=posf.t[:], in_=posi.t[:])
            for i in range(16):
                O(dve, "tensor_scalar", reads=[posf], writes=[ang], out=ang.t[:, :, i], in0=posf.t[:],
                  scalar1=float(np.float32(10000.0) ** np.float32(-(2.0 * i) / 32.0)), scalar2=None, op0=ALU.mult)
            C1 = 6.28125
            C2 = 2.0 * math.pi - C1
            for (tab, shift) in ((sin_t, 0.0), (cos_t, math.pi / 2.0)):
                O(dve, "tensor_scalar", reads=[ang], writes=[rk], out=rk.t[:], in0=ang.t[:], scalar1=shift,
                  scalar2=1.0 / (2.0 * math.pi), op0=ALU.add, op1=ALU.mult)
                O(dve, "tensor_copy", reads=[rk], writes=[rki], out=rki.t[:], in_=rk.t[:])
                O(dve, "tensor_copy", reads=[rki], writes=[rk], out=rk.t[:], in_=rki.t[:])
                O(dve, "scalar_tensor_tensor", reads=[rk, ang], writes=[rr_], out=rr_.t[:], in0=rk.t[:], scalar=-C1,
                  in1=ang.t[:], op0=ALU.mult, op1=ALU.add)
                O(dve, "scalar_tensor_tensor", reads=[rk, rr_], writes=[rr_], out=rr_.t[:], in0=rk.t[:], scalar=-C2,
                  in1=rr_.t[:], op0=ALU.mult, op1=ALU.add)
                if shift:
                    O(dve, "tensor_scalar", reads=[rr_], writes=[rr_], out=rr_.t[:], in0=rr_.t[:], scalar1=shift,
                      scalar2=None, op0=ALU.add)
                O(dve, "tensor_scalar", reads=[rr_], writes=[msk], out=msk.t[:], in0=rr_.t[:], scalar1=math.pi,
                  scalar2=-2.0 * math.pi, op0=ALU.is_gt, op1=ALU.mult)
                O(dve, "tensor_tensor", reads=[rr_, msk], writes=[rr_], out=rr_.t[:], in0=rr_.t[:], in1=msk.t[:], op=ALU.add)
                O(dve, "tensor_scalar", reads=[rr_], writes=[msk], out=msk.t[:], in0=rr_.t[:], scalar1=-math.pi,
                  scalar2=2.0 * math.pi, op0=ALU.is_lt, op1=ALU.mult)
                O(dve, "tensor_tensor", reads=[rr_, msk], writes=[rr_], out=rr_.t[:], in0=rr_.t[:], in1=msk.t[:], op=ALU.add)
                O(dve, "tensor_scalar", reads=[rr_], writes=[rr_], out=rr_.t[:], in0=rr_.t[:], scalar1=math.pi,
                  scalar2=-math.pi, op0=ALU.min, op1=ALU.max)
                O(act, "activation", reads=[rr_], writes=[tab], out=tab.t[:], in_=rr_.t[:], func=AF.Sin)
            adaln(range(0, 4), modA, 0, [(G_A, "norm_mix")])

        if debug == "S1":
            es.close()
            m.es.close()
            return
        xt = [tl(f"xt{i}", [128, D], F32, es) for i in range(3)]
        junk = tl("junk", [128, D], F32, es)
        hmid = tl("hmid", [128, D], F32, es)
        hb = [tl(f"hb{i}", [128, D], BF16, es) for i in range(2)]
        hTs = [tl(f"hT{i}", [128, 8, 512], BF16, es) for i in range(2)]
        st = [tl(f"st{i}", [128, 8], F32, es) for i in range(8)]
        junk4 = tl("junk4", [128, 256], F32, es)
        ub = [tl(f"ub{i}", [128, 4, 542], BF16, es) for i in range(2)]
        sig = [tl(f"sig{i}", [128, 512], F32, es) for i in range(2)]
        cqTs = [tl(f"cqT{i}", [128, 2, 512], BF16, es) for i in range(2)]
        ckvTs = [tl(f"ckvT{i}", [128, 512], BF16, es) for i in range(2)]
        dwb = tl("dwb", [128, 4, 512], F32, es)
        dw2 = [tl(f"dw2{i}", [128, 512], F32, es) for i in range(4)]
        mean = tl("mean", [128, 512], F32, es)
        rstd = tl("rstd", [128, 512], F32, es)
        lt = [tl(f"lt{i}", [128, 512], F32, es) for i in range(2)]
        yc = tl("yc", [128, 4, 512], BF16, es)
        sq = tl("sq", [128, 8, 96], F32, es)
        qn = tl("qn", [128, 8, 96], F32, es)
        kf = tl("kf", [128, 8, 96], F32, es)
        kn = tl("kn", [128, 8, 96], F32, es)
        rt = [tl(f"rt{i}", [128, 8, 16], F32, es) for i in range(4)]
        qfin = [tl(f"qfin{i}", [128, 8, 96], BF16, es) for i in range(2)]
        kfin = [tl(f"kfin{i}", [128, 8, 96], BF16, es) for i in range(2)]
        vblk = tl("vblk", [128, 8, 4, 64], BF16, es)
        qTb = tl("qTb", [128, 8, 512], BF16, es)
        kTb = tl("kTb", [128, 8, 512], BF16, es)
        d_x = [m.dsem(f"d_x{i}") for i in range(3)]
        d_sty, d_stq, d_stk, d_stv = (m.dsem(n) for n in ("d_sty", "d_stq", "d_stk", "d_stv"))

        for i in range(2):
            O(pool, "memset", writes=[ub[i]], ap=ub[i].t[:, :, 0:30], constant=0.0)

        def load_x(t):
            DMA(sp, d_x[t % 3], xt[t % 3].t[:], I["x"][t * 128:(t + 1) * 128, :], writes=[xt[t % 3]])

        load_x(0)
        load_x(1)
        pc_next = 0
        for b in range(NB if not (debug or "").startswith("A") or len(debug) == 1 else int(debug[1:])):
            for _ in range(4 if not int(os.environ.get("MK_NOPC", "0")) else 0):
                if pc_next < 32:
                    precast(pc_next, gate=[hTs[(b + 1) % 2].b] if b > 0 else ())
                    pc_next += 1
            m.begin_seg(b)
            u = ub[b % 2]
            up = ub[(b + 1) % 2]
            hT, cqT, ckvT = hTs[b % 2], cqTs[b % 2], ckvTs[b % 2]
            for r in range(4):
                t = b * 4 + r
                x = xt[t % 3]
                if t + 2 < NT:
                    load_x(t + 2)
                s = st[t % 8]
                O(act, "activation", reads=[x], writes=[junk, s], out=junk.t[:], in_=x.t[:], func=AF.Square,
                  accum_out=s.t[:, 0:1])
                O(act, "activation", reads=[s], writes=[s], out=s.t[:, 1:2], in_=s.t[:, 0:1], func=AF.Sqrt,
                  scale=1.0 / D, bias=EPS)
                O(dve, "reciprocal", reads=[s], writes=[s], out=s.t[:, 2:3], in_=s.t[:, 1:2])
                O(dve, "scalar_tensor_tensor", reads=[x, s, modA], writes=[hmid], out=hmid.t[:], in0=x.t[:],
                  scalar=s.t[:, 2:3], in1=G_A, op0=ALU.mult, op1=ALU.mult)
                h = hb[t % 2]
                O(pool, "tensor_tensor", reads=[hmid, modA], writes=[h], out=h.t[:], in0=hmid.t[:], in1=SH_A, op=ALU.add)
                pb = bank("tpA", [0])
                pbv = pb.t[:].bitcast(BF16).rearrange("p (k n) -> p k n", k=8)
                for k in range(8):
                    O(pe, "transpose", reads=[h, ident_b], writes=[pb], signal=(k == 7), out=pbv[:, k, :],
                      in_=h.t[:, k * 128:(k + 1) * 128], identity=ident_b.t[:])
                O(act if r % 2 == 0 else dve, "copy" if r % 2 == 0 else "tensor_copy", reads=[pb], writes=[hT],
                  out=hT.t[:, :, r * 128:(r + 1) * 128], in_=pbv)
            for j in range(4):
                pg = bank("mmA", [2, 3])
                for k in range(8):
                    O(pe, "matmul", reads=[win, hT], writes=[pg], signal=(k == 7), out=pg.t[:],
                      lhsT=win.t[:, k, 512 + j * 128:512 + (j + 1) * 128], rhs=hT.t[:, k, :], start=(k == 0), stop=(k == 7))
                sg = sig[j % 2]
                O(act, "activation", reads=[pg], writes=[sg], out=sg.t[:], in_=pg.t[:], func=AF.Sigmoid)
                pv = bank("mmA", [2, 3])
                for k in range(8):
                    O(pe, "matmul", reads=[win, hT], writes=[pv], signal=(k == 7), out=pv.t[:],
                      lhsT=win.t[:, k, j * 128:(j + 1) * 128], rhs=hT.t[:, k, :], start=(k == 0), stop=(k == 7))
                O(dve, "tensor_tensor", reads=[pv, sg], writes=[u], out=u.t[:, j, 30:542], in0=pv.t[:], in1=sg.t[:], op=ALU.mult)
            if b + 1 < NB:
                O(pool, "tensor_copy", reads=[u], writes=[up], out=up.t[:, :, 0:30], in_=u.t[:, :, 512:542])
            for j in range(3):
                pq = bank("mmA", [2, 3])
                for k in range(8):
                    O(pe, "matmul", reads=[win, hT], writes=[pq], signal=(k == 7), out=pq.t[:],
                      lhsT=win.t[:, k, 1024 + j * 128:1024 + (j + 1) * 128], rhs=hT.t[:, k, :], start=(k == 0), stop=(k == 7))
                if j < 2:
                    O(act, "copy", reads=[pq], writes=[cqT], out=cqT.t[:, j, :], in_=pq.t[:])
                else:
                    O(act, "copy", reads=[pq], writes=[ckvT], out=ckvT.t[:], in_=pq.t[:])
            pm = bank("stat", [6, 7])
            pm2 = bank("stat", [6, 7])
            for j in range(4):
                pc = bank("mmA", [2, 3])
                for k in range(31):
                    O(pe, "matmul", reads=[diag, u], writes=[pc], signal=(k == 30), out=pc.t[:], lhsT=diag.t[:, j, k, :],
                      rhs=u.t[:, j, k:k + 512], start=(k == 0), stop=(k == 30))
                O(act, "activation", reads=[pc, cb], writes=[dwb], out=dwb.t[:, j, :], in_=pc.t[:], func=AF.Identity,
                  bias=cb.t[:, j:j + 1])
                d2 = dw2[j]
                O(act, "activation", reads=[pc, cb], writes=[d2], out=d2.t[:], in_=pc.t[:], func=AF.Square,
                  bias=cb.t[:, j:j + 1])
            for j in range(4):
                O(pe, "matmul", reads=[avg_f, dwb], writes=[pm], signal=(j == 3), out=pm.t[:], lhsT=avg_f.t[:],
                  rhs=dwb.t[:, j, :], start=(j == 0), stop=(j == 3))
            for j in range(4):
                O(pe, "matmul", reads=[avg_f, dw2[j]], writes=[pm2], signal=(j == 3), out=pm2.t[:], lhsT=avg_f.t[:],
                  rhs=dw2[j].t[:], start=(j == 0), stop=(j == 3))
            O(act, "copy", reads=[pm], writes=[mean], out=mean.t[:], in_=pm.t[:])
            O(dve, "tensor_tensor", reads=[mean], writes=[rstd], out=rstd.t[:], in0=mean.t[:], in1=mean.t[:], op=ALU.mult)
            O(dve, "tensor_tensor", reads=[pm2, rstd], writes=[rstd], out=rstd.t[:], in0=pm2.t[:], in1=rstd.t[:], op=ALU.subtract)
            O(act, "activation", reads=[rstd], writes=[rstd], out=rstd.t[:], in_=rstd.t[:], func=AF.Sqrt, bias=EPS)
            O(dve, "reciprocal", reads=[rstd], writes=[rstd], out=rstd.t[:], in_=rstd.t[:])
            for j in range(4):
                l = lt[j % 2]
                O(dve, "tensor_tensor", reads=[dwb, mean], writes=[l], out=l.t[:], in0=dwb.t[:, j, :], in1=mean.t[:], op=ALU.subtract)
                O(pool, "tensor_tensor", reads=[l, rstd], writes=[l], out=l.t[:], in0=l.t[:], in1=rstd.t[:], op=ALU.mult)
                O(act, "activation", reads=[l, lng, lnb], writes=[yc], out=yc.t[:, j, :], in_=l.t[:], func=AF.Silu,
                  scale=lng.t[:, j:j + 1], bias=lnb.t[:, j:j + 1])
            DMA(sp, d_sty, YC_d[:, :, b * 512:(b + 1) * 512].rearrange("j p n -> p j n"), yc.t[:], reads=[yc], writes=[B_YC])
            m.end_seg()
            m.begin_seg(b + 1)
            for r in range(4):
                t = b * 4 + r
                s = st[t % 8]
                tok = slice(r * 128, (r + 1) * 128)
                pt = bank("mmB", [4, 5])
                for k in range(8):
                    O(pe, "matmul", reads=[win, hT], writes=[pt], signal=(k == 7), out=pt.t[:, 0:416], lhsT=hT.t[:, k, tok],
                      rhs=win.t[:, k, 1024:1440], start=(k == 0), stop=(k == 7))
                O(act, "activation", reads=[pt], writes=[junk4, s], out=junk4.t[:, 0:256], in_=pt.t[:, 0:256], func=AF.Square,
                  accum_out=s.t[:, 3:4])
                O(act, "activation", reads=[pt], writes=[junk4, s], out=junk4.t[:, 0:128], in_=pt.t[:, 256:384], func=AF.Square,
                  accum_out=s.t[:, 4:5])
                O(act, "activation", reads=[s], writes=[s], out=s.t[:, 5:6], in_=s.t[:, 3:4], func=AF.Sqrt, scale=1.0 / 256, bias=EPS)
                O(act, "activation", reads=[s], writes=[s], out=s.t[:, 6:7], in_=s.t[:, 4:5], func=AF.Sqrt, scale=1.0 / 128, bias=EPS)
                O(dve, "reciprocal", reads=[s], writes=[s], out=s.t[:, 5:7], in_=s.t[:, 5:7])
                O(act, "copy", reads=[pt], writes=[kf], out=kf.t[:, :, 64:96],
                  in_=pt.t[:, 384:416].unsqueeze(1).to_broadcast([128, 8, 32]))
                pq0 = bank("mmB", [4, 5])
                pq1 = bank("mmB", [4, 5])
                for n, pq in enumerate((pq0, pq1)):
                    for k in range(2):
                        O(pe, "matmul", reads=[cqT, wqb], writes=[pq], signal=(k == 1), out=pq.t[:, 0:384], lhsT=cqT.t[:, k, tok],
                          rhs=wqb.t[:, k, n * 384:(n + 1) * 384], start=(k == 0), stop=(k == 1))
                for n, pq in enumerate((pq0, pq1)):
                    O(act, "activation", reads=[pq, s], writes=[qn], out=qn.t[:, n * 4:(n + 1) * 4, :],
                      in_=pq.t[:, 0:384].rearrange("p (h d) -> p h d", h=4), func=AF.Identity, scale=s.t[:, 5:6])
                pk0 = bank("mmB", [4, 5])
                pk1 = bank("mmB", [4, 5])
                for n, pk in enumerate((pk0, pk1)):
                    O(pe, "matmul", reads=[ckvT, wkvb], writes=[pk], out=pk.t[:], lhsT=ckvT.t[:, tok],
                      rhs=wkvb.t[:, n * 512:(n + 1) * 512], start=True, stop=True)
                for n, pk in enumerate((pk0, pk1)):
                    pkv = pk.t[:].rearrange("p (h d) -> p h d", h=4)
                    O(act, "activation", reads=[pk, s], writes=[kf], out=kf.t[:, n * 4:(n + 1) * 4, 0:64], in_=pkv[:, :, 0:64],
                      func=AF.Identity, scale=s.t[:, 6:7])
                    O(dve, "tensor_scalar", reads=[pk, s], writes=[vblk], out=vblk.t[:, n * 4:(n + 1) * 4, r, :], in0=pkv[:, :, 64:128],
                      scalar1=s.t[:, 6:7], scalar2=None, op0=ALU.mult)
                for (src, dst, g, fin, so) in ((qn, qn, gq, qfin[t % 2], 0), (kf, kn, gk, kfin[t % 2], 1)):
                    ss = st[t % 8]
                    eng2 = dve if so == 0 else pool
                    O(eng2, "tensor_tensor", reads=[src], writes=[sq], out=sq.t[:], in0=src.t[:], in1=src.t[:], op=ALU.mult)
                    O(dve, "tensor_reduce", reads=[sq], writes=[rt[3]], out=rt[3].t[:, :, so], in_=sq.t[:], axis=AX.X, op=ALU.add)
                    O(act, "activation", reads=[rt[3]], writes=[rt[3]], out=rt[3].t[:, :, 2 + so], in_=rt[3].t[:, :, so], func=AF.Sqrt,
                      scale=1.0 / 96, bias=EPS)
                    O(dve, "reciprocal", reads=[rt[3]], writes=[rt[3]], out=rt[3].t[:, :, 4 + so], in_=rt[3].t[:, :, 2 + so])
                    O(dve, "tensor_tensor", reads=[src, rt[3]], writes=[dst], out=dst.t[:], in0=src.t[:],
                      in1=rt[3].t[:, :, 4 + so:5 + so].to_broadcast([128, 8, 96]), op=ALU.mult)
                    O(eng2, "tensor_tensor", reads=[dst, g], writes=[dst], out=dst.t[:], in0=dst.t[:], in1=g.t[:], op=ALU.mult)
                    O(act, "copy", reads=[dst], writes=[fin], out=fin.t[:, :, 0:64], in_=dst.t[:, :, 0:64])
                    cosb = cos_t.t[:, t:t + 1, :].to_broadcast([128, 8, 16])
                    sinb = sin_t.t[:, t:t + 1, :].to_broadcast([128, 8, 16])
                    x1 = dst.t[:, :, 64:80]
                    x2 = dst.t[:, :, 80:96]
                    O(pool, "tensor_tensor", reads=[dst, cos_t], writes=[rt[0]], out=rt[0].t[:], in0=x1, in1=cosb, op=ALU.mult)
                    O(pool, "tensor_tensor", reads=[dst, sin_t], writes=[rt[1]], out=rt[1].t[:], in0=x2, in1=sinb, op=ALU.mult)
                    O(pool, "tensor_tensor", reads=[rt[0], rt[1]], writes=[fin], out=fin.t[:, :, 64:80], in0=rt[0].t[:], in1=rt[1].t[:],
                      op=ALU.subtract)
                    O(dve, "tensor_tensor", reads=[dst, sin_t], writes=[rt[0]], out=rt[0].t[:], in0=x1, in1=sinb, op=ALU.mult)
                    O(dve, "tensor_tensor", reads=[dst, cos_t], writes=[rt[1]], out=rt[1].t[:], in0=x2, in1=cosb, op=ALU.mult)
                    O(dve, "tensor_tensor", reads=[rt[0], rt[1]], writes=[fin], out=fin.t[:, :, 80:96], in0=rt[0].t[:], in1=rt[1].t[:],
                      op=ALU.add)
                for (fin, dstT) in ((qfin[t % 2], qTb), (kfin[t % 2], kTb)):
                    pb = bank("tpB", [1])
                    pbv = pb.t[:].bitcast(BF16).rearrange("p (k n) -> p k n", k=8)
                    for h in range(8):
                        O(pe, "transpose", reads=[fin, ident_b], writes=[pb], signal=(h == 7), out=pbv[0:96, h, :],
                          in_=fin.t[:, h, :], identity=ident_b.t[:])
                    O(dve, "tensor_copy", reads=[pb], writes=[dstT], out=dstT.t[0:96, :, tok], in_=pbv[0:96, :, :])
            DMA(sp, d_stq, QT_d[:, :, b * 512:(b + 1) * 512].rearrange("h p n -> p h n"), qTb.t[0:96, :, :], reads=[qTb], writes=[B_QT])
            DMA(act, d_stk, KT_d[:, :, b * 512:(b + 1) * 512].rearrange("h p n -> p h n"), kTb.t[0:96, :, :], reads=[kTb], writes=[B_KT])
            DMA(sp, d_stv, V_d[:, :, b * 4:(b + 1) * 4, :].rearrange("h p j d -> p h j d"), vblk.t[:], reads=[vblk], writes=[B_V])
            m.end_seg()
        while pc_next < 32:
            precast(pc_next)
            pc_next += 1
        m.barrier()

    if dA:
        m.es.close()
        return nc

    modB = tl("modB", [128, 4 * D], F32)
    GT_A, SH_F, G_F, GT_F = (modB.t[:, i * D:(i + 1) * D] for i in range(4))
    d_wo = m.dsem("d_wo")
    wout = tl("wout", [128, 8, 1024], BF16)
    m.dma(pool, d_wo, lambda: pool.h.dma_start(out=wout.t[:], in_=I["w_out"].rearrange("(k p) n -> p k n", p=128)),
          writes=[wout.b])
    with ExitStack() as es:
        ada_chunk, ada_finish = adaln_parts("_b", es, [0, 1], dve)
        QTh = [tl(f"QTh{i}", [128, S], BF16, es) for i in range(2)]
        KTh = [tl(f"KTh{i}", [128, S], BF16, es) for i in range(2)]
        Vh = [tl(f"Vh{i}", [128, NT, 128], BF16, es) for i in range(2)]
        PT = [tl(f"PT{i}", [128, 512], BF16, es) for i in range(6)]
        PTd = [tl(f"PTd{i}", [128, 512], BF16, es) for i in range(8)]
        Un = [tl(f"Un{i}", [128, 512], F32, es) for i in range(2)]
        Rc = [tl(f"Rc{i}", [128, 512], F32, es) for i in range(2)]
        R2 = [tl(f"R2{i}", [128, 512], F32, es) for i in range(2)]
        Yh = [tl(f"Yh{i}", [128, 512], BF16, es) for i in range(2)]
        d_q = [m.dsem(f"d_q{i}") for i in range(2)]
        d_k = [m.dsem(f"d_k{i}") for i in range(2)]
        d_v = [m.dsem(f"d_v{i}") for i in range(2)]
        d_r2 = [m.dsem(f"d_r2{i}") for i in range(2)]
        d_ya = [m.dsem(f"d_ya{i}") for i in range(2)]
        for i in range(2):
            O(pool, "memset", writes=[Vh[i]], ap=Vh[i].t[:, :, 64:128], constant=1.0)
            O(pool, "memset", writes=[KTh[i]], ap=KTh[i].t[64:128, :], constant=1.0)
            O(dve, "tensor_scalar", reads=[KTh[i], negM], writes=[QTh[i]], out=QTh[i].t[64:128, :], in0=KTh[i].t[64:128, :],
              scalar1=negM.t[64:128, 0:1], scalar2=None, op0=ALU.mult)

        def load_head(h):
            i = h % 2
            DMA(sp, d_q[i], QTh[i].t[0:96, :], QT_d[h], reads=[B_QT], writes=[QTh[i]])
            DMA(sp, d_k[i], KTh[i].t[0:96, :], KT_d[h], reads=[B_KT], writes=[KTh[i]])
            DMA(act, d_v[i], Vh[i].t[:, :, 0:64], V_d[h], reads=[B_V], writes=[Vh[i]])

        load_head(0)
        nq = 0
        for h in range(8):
            if h + 1 < 8:
                load_head(h + 1)
            Q, Kt, V = QTh[h % 2], KTh[h % 2], Vh[h % 2]
            for qb in range(NB):
                if h == 0:
                    ada_chunk(4 + qb, modB, 4, gate=[Yh[(nq + 1) % 2]] if qb > 0 else (), wlat=40.0)
                    if qb == NB - 1:
                        ada_finish(modB, [(G_F, "norm_ffn")])
                po = bank("o", [6, 7])
                njt = 4 * qb + 4
                pend = []

                def emit_s(j):
                    r = j - 4 * qb
                    c0 = 128 * max(r, 0)
                    n = 512 - c0
                    ps = bank("s", [2, 3, 4, 5])
                    O(pe, "matmul", reads=[Kt, Q], writes=[ps], out=ps.t[:, 0:n], lhsT=Kt.t[0:97, j * 128:(j + 1) * 128],
                      rhs=Q.t[0:97, qb * 512 + c0:(qb + 1) * 512], start=True, stop=True)
                    if r >= 0:
                        pt = PTd[(4 * (nq % 2)) + r]
                        O(act, "activation", reads=[ps], writes=[pt], out=pt.t[:, 0:n], in_=ps.t[:, 0:n], func=AF.Exp)
                        O(pool, "memset", writes=[pt], ap=pt.t[64:128, 0:64], constant=0.0)
                    else:
                        pt = bank_pt()
                        O(act, "activation", reads=[ps], writes=[pt], out=pt.t[:, 0:n], in_=ps.t[:, 0:n], func=AF.Exp)
                    return (j, pt, c0, n)

                def bank_pt():
                    i = rr.get("pt", 0)
                    rr["pt"] = i + 1
                    return PT[i % 6]

                def emit_pv(item):
                    j, pt, c0, n = item
                    O(pe, "matmul", reads=[V, pt], writes=[po], signal=(j == njt - 1), out=po.t[:, c0:512], lhsT=V.t[:, j, :],
                      rhs=pt.t[:, 0:n], start=(j == 0), stop=(j == njt - 1))

                LOOK = 3
                for j in range(njt):
                    pend.append(emit_s(j))
                    if len(pend) > LOOK:
                        emit_pv(pend.pop(0))
                while pend:
                    emit_pv(pend.pop(0))
                i = nq % 2
                nq += 1
                O(dve, "tensor_copy", reads=[po], writes=[Un[i]], out=Un[i].t[0:64, :], in_=po.t[0:64, :])
                O(dve, "reciprocal", reads=[po], writes=[Rc[i]], out=Rc[i].t[64:128, :], in_=po.t[64:128, :])
                DMA(sp, d_r2[i], R2[i].t[0:64, :], Rc[i].t[64:128, :], reads=[Rc[i]], writes=[R2[i]])
                O(pool, "tensor_tensor", reads=[Un[i], R2[i]], writes=[Yh[i]], out=Yh[i].t[0:64, :], in0=Un[i].t[0:64, :],
                  in1=R2[i].t[0:64, :], op=ALU.mult)
                DMA(sp, d_ya[i], YA_d[h, :, qb * 512:(qb + 1) * 512], Yh[i].t[0:64, :], reads=[Yh[i]], writes=[B_YA])
        m.barrier()

    if debug == "B1":
        m.es.close()
        return nc

    SP_E = [mybir.EngineType.SP]
    OH = tl("OH", [128, NT, 2, 32], F32)
    Wt = tl("Wt", [128, NT, 2], F32)
    PF = tl("PF", [128, NT, 2], F32)
    Ssum = tl("Ssum", [128, 32], F32)
    EOFF = tl("EOFF", [128, 32], F32)
    ustr = tl("ustr", [128, 128], F32)
    O(pool, "memset", writes=[Ssum], ap=Ssum.t[:], constant=0.0)
    for e in range(32):
        O(pool, "memset", writes=[EOFF], ap=EOFF.t[:, e:e + 1], constant=float(e * ECAP))
    O(pool, "memset", writes=[ustr], ap=ustr.t[:], constant=1.0)
    O(pool, "affine_select", reads=[ustr], writes=[ustr], out=ustr.t[:], in_=ustr.t[:], pattern=[[1, 128]],
      compare_op=ALU.is_gt, fill=0.0, base=0, channel_multiplier=-1)
    with ExitStack() as es:
        wr = tl("wr", [128, 8, 36], F32, es)
        br = tl("br", [1, 36], F32, es)
        ycat = [tl(f"ycat{i}", [128, 8, 512], BF16, es) for i in range(2)]
        xr = [tl(f"xr{i}", [128, D], F32, es) for i in range(2)]
        x1 = [tl(f"x1{i}", [128, D], F32, es) for i in range(2)]
        h2b = [tl(f"h2b{i}", [128, D], BF16, es) for i in range(2)]
        sm = [tl(f"sm{i}", [128, 16], F32, es) for i in range(2)]
        tmp2 = {}
        for nm_, sh_ in (("junkb", [128, D]), ("hm", [128, D]), ("h2f", [128, D]), ("h2T", [128, 8, 128]), ("L", [128, 36]),
                         ("goh", [128, 4]), ("gex", [128, 4]), ("t48", [128, 4, 8]), ("ein", [128, 8]), ("oh1", [128, 8]),
                         ("oh2", [128, 8]), ("msk8", [128, 8]), ("St", [128, 32]), ("Cb", [128, 32]), ("t32", [128, 32])):
            tmp2[nm_] = [tl(f"{nm_}{i}", sh_, F32, es) for i in range(2)]
        idx = [tl(f"idx{i}", [128, 2], I32, es) for i in range(2)]
        d_yc = [m.dsem(f"d_yc{i}") for i in range(2)]
        d_xr = [m.dsem(f"d_xr{i}") for i in range(2)]
        d_x1 = [m.dsem(f"d_x1{i}") for i in range(2)]
        d_sc = [m.dsem(f"d_sc{i}") for i in range(2)]
        d_wr = m.dsem("d_wr")
        DMA(sp, d_wr, wr.t[:], I["w_router"].rearrange("(k p) n -> p k n", p=128), writes=[wr])
        DMA(sp, d_wr, br.t[:], I["b_router"], writes=[br])
        YAv = YA_d.rearrange("(a e) p n -> e p a n", e=2)

        def load_ycat(b):
            y = ycat[b % 2]
            cols = slice(b * 512, (b + 1) * 512)
            DMA(sp, d_yc[b % 2], y.t[:, 0:4, :], YC_d[:, :, cols].rearrange("j p n -> p j n"), reads=[B_YC], writes=[y])
            for e in range(2):
                DMA(act, d_yc[b % 2], y.t[e * 64:(e + 1) * 64, 4:8, :], YAv[e][:, :, cols], reads=[B_YA], writes=[y])

        def load_xr(t):
            DMA(sp, d_xr[t % 2], xr[t % 2].t[:], I["x"][t * 128:(t + 1) * 128, :], writes=[xr[t % 2]])

        load_ycat(0)
        load_xr(0)
        for b in range(NB):
            if b + 1 < NB:
                load_ycat(b + 1)
            y = ycat[b % 2]
            for r in range(4):
                t = b * 4 + r
                tok = slice(r * 128, (r + 1) * 128)
                if t + 1 < NT:
                    load_xr(t + 1)
                x = xr[t % 2]
                xo = x1[t % 2]
                s = sm[t % 2]
                junk, hm, h2f, h2T, L, goh, gex, t48, ein, oh1, oh2, msk8, St, Cb, t32 = (tmp2[nm_][t % 2] for nm_ in (
                    "junkb", "hm", "h2f", "h2T", "L", "goh", "gex", "t48", "ein", "oh1", "oh2", "msk8", "St", "Cb", "t32"))
                for hf in range(2):
                    pm = bank("mm", [2, 3, 4, 5])
                    for k in range(8):
                        O(pe, "matmul", reads=[y, wout], writes=[pm], signal=(k == 7), out=pm.t[:], lhsT=y.t[:, k, tok],
                          rhs=wout.t[:, k, hf * 512:(hf + 1) * 512], start=(k == 0), stop=(k == 7))
                    O(dve, "tensor_tensor", reads=[pm, modB], writes=[xo], out=xo.t[:, hf * 512:(hf + 1) * 512], in0=pm.t[:],
                      in1=GT_A[:, hf * 512:(hf + 1) * 512], op=ALU.mult)
                O(pool, "tensor_tensor", reads=[xo, x], writes=[xo], out=xo.t[:], in0=xo.t[:], in1=x.t[:], op=ALU.add)
                DMA(sp, d_x1[t % 2], out_d[t * 128:(t + 1) * 128, :], xo.t[:], reads=[xo], writes=[B_OUT])
                O(act, "activation", reads=[xo], writes=[junk, s], out=junk.t[:], in_=xo.t[:], func=AF.Square, accum_out=s.t[:, 0:1])
                O(act, "activation", reads=[s], writes=[s], out=s.t[:, 1:2], in_=s.t[:, 0:1], func=AF.Sqrt, scale=1.0 / D, bias=EPS)
                O(dve, "reciprocal", reads=[s], writes=[s], out=s.t[:, 2:3], in_=s.t[:, 1:2])
                O(dve, "scalar_tensor_tensor", reads=[xo, s, modB], writes=[hm], out=hm.t[:], in0=xo.t[:], scalar=s.t[:, 2:3],
                  in1=G_F, op0=ALU.mult, op1=ALU.mult)
                O(pool, "tensor_tensor", reads=[hm, modB], writes=[h2f], out=h2f.t[:], in0=hm.t[:], in1=SH_F, op=ALU.add)
                hb2 = h2b[t % 2]
                O(act, "copy", reads=[h2f], writes=[hb2], out=hb2.t[:], in_=h2f.t[:])
                for hf in range(2):
                    pb = bank("tp", [0, 1])
                    pbv = pb.t[:].rearrange("p (k n) -> p k n", k=4)
                    for k in range(4):
                        kk = hf * 4 + k
                        O(pe, "transpose", reads=[h2f, ident_f], writes=[pb], signal=(k == 3), out=pbv[:, k, :],
                          in_=h2f.t[:, kk * 128:(kk + 1) * 128], identity=ident_f.t[:])
                    O(act, "copy", reads=[pb], writes=[h2T], out=h2T.t[:, hf * 4:(hf + 1) * 4, :], in_=pbv)
                pl = bank("stat", [6, 7])
                for k in range(8):
                    O(pe, "matmul", reads=[h2T, wr], writes=[pl], signal=False, out=pl.t[:, 0:36], lhsT=h2T.t[:, k, :], rhs=wr.t[:, k, :],
                      start=(k == 0), stop=False)
                O(pe, "matmul", reads=[ones_f, br], writes=[pl], out=pl.t[:, 0:36], lhsT=ones_f.t[0:1, :], rhs=br.t[0:1, :],
                  start=False, stop=True)
                O(act, "copy", reads=[pl], writes=[L], out=L.t[:], in_=pl.t[:, 0:36])
                O(dve, "tensor_reduce", reads=[L], writes=[s], out=s.t[:, 3:4], in_=L.t[:, 0:4], axis=AX.X, op=ALU.max)
                O(dve, "tensor_scalar", reads=[L, s], writes=[goh], out=goh.t[:], in0=L.t[:, 0:4], scalar1=s.t[:, 3:4], scalar2=None,
                  op0=ALU.is_equal)
                O(dve, "tensor_scalar", reads=[s], writes=[s], out=s.t[:, 4:5], in0=s.t[:, 3:4], scalar1=-1.0, scalar2=None, op0=ALU.mult)
                O(act, "activation", reads=[L, s], writes=[gex, s], out=gex.t[:], in_=L.t[:, 0:4], func=AF.Exp, bias=s.t[:, 4:5],
                  accum_out=s.t[:, 5:6])
                O(dve, "reciprocal", reads=[s], writes=[s], out=s.t[:, 6:7], in_=s.t[:, 5:6])
                O(dve, "tensor_tensor", reads=[L, goh], writes=[t48], out=t48.t[:], in0=L.t[:, 4:36].rearrange("p (g e) -> p g e", g=4),
                  in1=goh.t[:].unsqueeze(2).to_broadcast([128, 4, 8]), op=ALU.mult)
                O(dve, "tensor_reduce", reads=[t48], writes=[ein], out=ein.t[:], in_=t48.t[:].rearrange("p g e -> p e g"), axis=AX.X,
                  op=ALU.add)
                O(dve, "tensor_reduce", reads=[ein], writes=[s], out=s.t[:, 7:8], in_=ein.t[:], axis=AX.X, op=ALU.max)
                O(dve, "tensor_scalar", reads=[ein, s], writes=[oh1], out=oh1.t[:], in0=ein.t[:], scalar1=s.t[:, 7:8], scalar2=None,
                  op0=ALU.is_equal)
                O(dve, "scalar_tensor_tensor", reads=[oh1, ein], writes=[msk8], out=msk8.t[:], in0=oh1.t[:], scalar=-1e30, in1=ein.t[:],
                  op0=ALU.mult, op1=ALU.add)
                O(dve, "tensor_reduce", reads=[msk8], writes=[s], out=s.t[:, 8:9], in_=msk8.t[:], axis=AX.X, op=ALU.max)
                O(dve, "tensor_scalar", reads=[msk8, s], writes=[oh2], out=oh2.t[:], in0=msk8.t[:], scalar1=s.t[:, 8:9], scalar2=None,
                  op0=ALU.is_equal)
                O(dve, "tensor_tensor", reads=[s], writes=[s], out=s.t[:, 9:10], in0=s.t[:, 8:9], in1=s.t[:, 7:8], op=ALU.subtract)
                O(act, "activation", reads=[s], writes=[s], out=s.t[:, 10:11], in_=s.t[:, 9:10], func=AF.Exp)
                O(dve, "tensor_scalar", reads=[s], writes=[s], out=s.t[:, 11:12], in0=s.t[:, 10:11], scalar1=1.0, scalar2=None, op0=ALU.add)
                O(dve, "reciprocal", reads=[s], writes=[s], out=s.t[:, 12:13], in_=s.t[:, 11:12])
                O(dve, "tensor_tensor", reads=[s], writes=[Wt], out=Wt.t[:, t, 0:1], in0=s.t[:, 12:13], in1=s.t[:, 6:7], op=ALU.mult)
                O(dve, "tensor_tensor", reads=[s, Wt], writes=[Wt], out=Wt.t[:, t, 1:2], in0=s.t[:, 6:7], in1=Wt.t[:, t, 0:1], op=ALU.subtract)
                for kk, oh in enumerate((oh1, oh2)):
                    O(dve, "tensor_tensor", reads=[goh, oh], writes=[OH], out=OH.t[:, t, kk, :].rearrange("p (g e) -> p g e", g=4),
                      in0=goh.t[:].unsqueeze(2).to_broadcast([128, 4, 8]), in1=oh.t[:].unsqueeze(1).to_broadcast([128, 4, 8]), op=ALU.mult)
                O(dve, "tensor_tensor", reads=[OH], writes=[St], out=St.t[:], in0=OH.t[:, t, 0, :], in1=OH.t[:, t, 1, :], op=ALU.add)
                pc_ = bank("stat", [6, 7])
                O(pe, "matmul", reads=[ustr, St], writes=[pc_], signal=False, out=pc_.t[:, 0:32], lhsT=ustr.t[:], rhs=St.t[:], start=True, stop=False)
                O(pe, "matmul", reads=[ones_f, Ssum], writes=[pc_], out=pc_.t[:, 0:32], lhsT=ones_f.t[:], rhs=Ssum.t[:], start=False, stop=True)
                O(dve, "tensor_tensor", reads=[pc_, EOFF], writes=[Cb], out=Cb.t[:], in0=pc_.t[:, 0:32], in1=EOFF.t[:], op=ALU.add)
                O(pool, "tensor_tensor", reads=[Ssum, St], writes=[Ssum], out=Ssum.t[:], in0=Ssum.t[:], in1=St.t[:], op=ALU.add)
                for kk in range(2):
                    O(dve, "tensor_tensor", reads=[OH, Cb], writes=[t32], out=t32.t[:], in0=OH.t[:, t, kk, :], in1=Cb.t[:], op=ALU.mult)
                    O(dve, "tensor_reduce", reads=[t32], writes=[PF], out=PF.t[:, t, kk:kk + 1], in_=t32.t[:], axis=AX.X, op=ALU.add)
                ix = idx[t % 2]
                O(dve, "tensor_copy", reads=[PF], writes=[ix], out=ix.t[:], in_=PF.t[:, t, :])
                for kk in range(2):
                    m.dma(pool, d_sc[t % 2], lambda ix=ix, kk=kk, hb2=hb2: pool.h.indirect_dma_start(
                        out=XS_d, out_offset=bass.IndirectOffsetOnAxis(ap=ix.t[:, kk:kk + 1], axis=0), in_=hb2.t[:], in_offset=None),
                        reads=[hb2.b, ix.b], writes=[B_XS], par=True)
        m.barrier()

    if debug == "B2":
        dbg = nc.dram_tensor("dbg", [128, NT, 6], F32, kind="ExternalOutput").ap()
        d_dbg = m.dsem("d_dbg")
        DMA(sp, d_dbg, dbg[:, :, 0:2], Wt.t[:], reads=[Wt], writes=[B_OUT])
        DMA(sp, d_dbg, dbg[:, :, 2:4], PF.t[:], reads=[PF], writes=[B_OUT])
        m.barrier()
        m.es.close()
        return nc

    ADJ = tl("ADJ", [128, 32], F32)
    with ExitStack() as es:
        cnt = tl("cnt", [128, 32], F32, es)
        cnti = tl("cnti", [128, 32], I32, es)
        ntl = tl("ntl", [128, 32], F32, es)
        tbi = tl("tbi", [128, 32], F32, es)
        tb = tl("tb", [128, 32], F32, es)
        jg = tl("jg", [128, NTILE], F32, es)
        cmp3 = tl("cmp3", [128, NTILE, 32], F32, es)
        ej = tl("ej", [128, NTILE], F32, es)
        sj = tl("sj", [128, NTILE], F32, es)
        rowf = tl("rowf", [128, NTILE], F32, es)
        tabi = tl("tabi", [128, 2, NTILE], I32, es)
        pcn = bank("stat", [6, 7])
        O(pe, "matmul", reads=[ones_f, Ssum], writes=[pcn], out=pcn.t[:, 0:32], lhsT=ones_f.t[:], rhs=Ssum.t[:], start=True, stop=True)
        O(dve, "tensor_scalar", reads=[pcn], writes=[cnt], out=cnt.t[:], in0=pcn.t[:, 0:32], scalar1=127.0, scalar2=None, op0=ALU.add)
        O(dve, "tensor_copy", reads=[cnt], writes=[cnti], out=cnti.t[:], in_=cnt.t[:])
        O(dve, "tensor_scalar", reads=[cnti], writes=[cnti], out=cnti.t[:], in0=cnti.t[:], scalar1=7, scalar2=None,
          op0=ALU.arith_shift_right)
        O(dve, "tensor_copy", reads=[cnti], writes=[ntl], out=ntl.t[:], in_=cnti.t[:])
        tb2 = tl("tb2", [128, 32], F32, es)
        src_, dst_ = ntl, tb2
        for sh in (1, 2, 4, 8, 16):
            O(dve, "tensor_tensor", reads=[src_], writes=[dst_], out=dst_.t[:, sh:32], in0=src_.t[:, sh:32], in1=src_.t[:, 0:32 - sh], op=ALU.add)
            O(act, "copy", reads=[src_], writes=[dst_], par=True, out=dst_.t[:, 0:sh], in_=src_.t[:, 0:sh])
            src_, dst_ = dst_, (tbi if dst_ is tb2 else tb2)
        if src_ is not tbi:
            O(dve, "tensor_copy", reads=[src_], writes=[tbi], out=tbi.t[:], in_=src_.t[:])
        O(dve, "tensor_tensor", reads=[tbi, ntl], writes=[tb], out=tb.t[:], in0=tbi.t[:], in1=ntl.t[:], op=ALU.subtract)
        O(dve, "scalar_tensor_tensor", reads=[tb, EOFF], writes=[ADJ], out=ADJ.t[:], in0=tb.t[:], scalar=-128.0, in1=EOFF.t[:],
          op0=ALU.mult, op1=ALU.add)
        jgi = tl("jgi", [128, NTILE], I32, es)
        O(pool, "iota", writes=[jgi], out=jgi.t[:], pattern=[[1, NTILE]], base=0, channel_multiplier=0)
        O(dve, "tensor_copy", reads=[jgi], writes=[jg], out=jg.t[:], in_=jgi.t[:])
        tbi_b = tbi.t[:].unsqueeze(1).to_broadcast([128, NTILE, 32])
        O(dve, "tensor_tensor", reads=[tbi, jg], writes=[cmp3], out=cmp3.t[:], in0=tbi_b,
          in1=jg.t[:].unsqueeze(2).to_broadcast([128, NTILE, 32]), op=ALU.is_le)
        O(dve, "tensor_reduce", reads=[cmp3], writes=[ej], out=ej.t[:], in_=cmp3.t[:], axis=AX.X, op=ALU.add)
        O(dve, "tensor_tensor", reads=[cmp3, tbi], writes=[cmp3], out=cmp3.t[:], in0=cmp3.t[:], in1=tbi_b, op=ALU.mult)
        O(dve, "tensor_reduce", reads=[cmp3], writes=[sj], out=sj.t[:], in_=cmp3.t[:], axis=AX.X, op=ALU.max)
        inv = tl("inv", [128, NTILE], F32, es)
        O(dve, "tensor_scalar", reads=[ej], writes=[inv], out=inv.t[:], in0=ej.t[:], scalar1=31.5, scalar2=float(2 ** 30),
          op0=ALU.is_gt, op1=ALU.mult)
        O(dve, "tensor_scalar", reads=[ej], writes=[ej], out=ej.t[:], in0=ej.t[:], scalar1=31.0, scalar2=None, op0=ALU.min)
        O(dve, "tensor_tensor", reads=[jg, sj], writes=[rowf], out=rowf.t[:], in0=jg.t[:], in1=sj.t[:], op=ALU.subtract)
        O(dve, "tensor_scalar", reads=[rowf], writes=[rowf], out=rowf.t[:], in0=rowf.t[:], scalar1=128.0, scalar2=None, op0=ALU.mult)
        O(dve, "scalar_tensor_tensor", reads=[ej, rowf], writes=[rowf], out=rowf.t[:], in0=ej.t[:], scalar=float(ECAP), in1=rowf.t[:],
          op0=ALU.mult, op1=ALU.add)
        O(dve, "tensor_scalar", reads=[rowf], writes=[rowf], out=rowf.t[:], in0=rowf.t[:], scalar1=float(32 * ECAP - 128), scalar2=0.0,
          op0=ALU.min, op1=ALU.max)
        pidi = tl("pidi", [128, NTILE], I32, es)
        pidf = tl("pidf", [128, NTILE], F32, es)
        O(pool, "iota", writes=[pidi], out=pidi.t[:], pattern=[[0, NTILE]], base=0, channel_multiplier=1)
        O(dve, "tensor_copy", reads=[pidi], writes=[pidf], out=pidf.t[:], in_=pidi.t[:])
        O(dve, "scalar_tensor_tensor", reads=[ej, pidf], writes=[ej], out=ej.t[:], in0=ej.t[:], scalar=128.0, in1=pidf.t[:],
          op0=ALU.mult, op1=ALU.add)
        O(dve, "tensor_tensor", reads=[rowf, pidf], writes=[rowf], out=rowf.t[:], in0=rowf.t[:], in1=pidf.t[:], op=ALU.add)
        O(dve, "tensor_tensor", reads=[ej, inv], writes=[ej], out=ej.t[:], in0=ej.t[:], in1=inv.t[:], op=ALU.add)
        O(dve, "tensor_tensor", reads=[rowf, inv], writes=[rowf], out=rowf.t[:], in0=rowf.t[:], in1=inv.t[:], op=ALU.add)
        O(dve, "tensor_copy", reads=[ej], writes=[tabi], out=tabi.t[:, 0, :], in_=ej.t[:])
        O(dve, "tensor_copy", reads=[rowf], writes=[tabi], out=tabi.t[:, 1, :], in_=rowf.t[:])
        m.barrier()

        regW = nc.alloc_register(mybir.EngineType.Pool, "bndW")
        regX = nc.alloc_register(mybir.EngineType.Pool, "bndX")
        nc.gpsimd.reg_mov(regW, 32 * 128 - 1)
        nc.gpsimd.reg_mov(regX, 32 * ECAP - 1)
        NBUF = 4
        NW = 3
        xg = [tl(f"xg{i}", [128, D], BF16, es) for i in range(NBUF)]
        wall = [tl(f"wall{i}", [128, 6144], BF16, es) for i in range(NBUF)]
        wg = [Tl(None) for _ in range(NBUF)]
        wu = [Tl(None) for _ in range(NBUF)]
        wd = [Tl(None) for _ in range(NBUF)]
        for i_ in range(NBUF):
            wg[i_].t = wall[i_].t[:, 0:2048].rearrange("p (k f) -> p k f", k=8)
            wu[i_].t = wall[i_].t[:, 2048:4096].rearrange("p (k f) -> p k f", k=8)
            wd[i_].t = wall[i_].t[:, 4096:6144].rearrange("p (j n) -> p j n", j=2)
            wg[i_].b = wu[i_].b = wd[i_].b = wall[i_].b
        xT = [tl(f"xT{i}", [128, 8, 128], BF16, es) for i in range(NW)]
        sgl = [tl(f"sgl{i}", [128, 2, 128], F32, es) for i in range(NW)]
        aT = [tl(f"aT{i}", [128, 2, 128], BF16, es) for i in range(NW)]
        ysb = [tl(f"ysb{i}", [128, D], F32, es) for i in range(NW)]
        d_mx = [m.dsem(f"d_mx{i}") for i in range(NBUF)]
        d_mw = [m.dsem(f"d_mw{i}") for i in range(NBUF)]
        d_ys = [m.dsem(f"d_ys{i}") for i in range(NW)]
        def load_tile(j):
            i = j % NBUF
            m.dma(pool, d_mw[i], lambda: pool.h.indirect_dma_start(
                out=wall[i].t[:], out_offset=None, in_=WA_d, in_offset=bass.IndirectOffsetOnAxis(ap=tabi.t[:, 0, j:j + 1], axis=0),
                bounds_check=regW, oob_is_err=False),
                reads=[B_WE, tabi.b], writes=[wall[i].b])
            m.dma(pool, d_mx[i], lambda: pool.h.indirect_dma_start(
                out=xg[i].t[:], out_offset=None, in_=XS_d, in_offset=bass.IndirectOffsetOnAxis(ap=tabi.t[:, 1, j:j + 1], axis=0),
                bounds_check=regX, oob_is_err=False),
                reads=[B_XS, tabi.b], writes=[xg[i].b])

        for j in range(min(NBUF - 1, NTILE)):
            load_tile(j)
        for j in range(NTILE):
            if j + NBUF - 1 < NTILE:
                load_tile(j + NBUF - 1)
            i = j % NBUF
            pb = bank("tp", [0, 1])
            pbv = pb.t[:].bitcast(BF16).rearrange("p (k n) -> p k n", k=8)
            for k in range(8):
                O(pe, "transpose", reads=[xg[i], ident_b], writes=[pb], signal=(k == 7), out=pbv[:, k, :], in_=xg[i].t[:, k::8],
                  identity=ident_b.t[:])
            xt_ = xT[j % NW]
            O(dve, "tensor_copy", reads=[pb], writes=[xt_], out=xt_.t[:], in_=pbv)
            pgu = bank("mmoe", [2, 3, 4, 5, 6, 7])
            pguv = pgu.t[:].rearrange("p (c n) -> p c n", c=4)
            for c in range(4):
                w_ = wg[i] if c < 2 else wu[i]
                fc = c % 2
                for k in range(8):
                    O(pe, "matmul", reads=[w_, xt_], writes=[pgu], signal=(c == 3 and k == 7), out=pguv[:, c, :],
                      lhsT=w_.t[:, k, fc * 128:(fc + 1) * 128], rhs=xt_.t[:, k, :], start=(k == 0), stop=(k == 7))
            sg_ = sgl[j % NW]
            a_ = aT[j % NW]
            O(act, "activation", reads=[pgu], writes=[sg_], out=sg_.t[:], in_=pguv[:, 0:2, :], func=AF.Silu)
            O(dve, "tensor_tensor", reads=[pgu, sg_], writes=[a_], out=a_.t[:], in0=pguv[:, 2:4, :], in1=sg_.t[:], op=ALU.mult)
            ys_ = ysb[j % NW]
            for hf in range(2):
                py = bank("mmoe", [2, 3, 4, 5, 6, 7])
                for jc in range(2):
                    O(pe, "matmul", reads=[a_, wd[i]], writes=[py], signal=(jc == 1), out=py.t[:], lhsT=a_.t[:, jc, :],
                      rhs=wd[i].t[:, jc, hf * 512:(hf + 1) * 512], start=(jc == 0), stop=(jc == 1))
                if hf == 0:
                    O(act, "copy", reads=[py], writes=[ys_], out=ys_.t[:, 0:512], in_=py.t[:])
                else:
                    O(dve, "tensor_copy", reads=[py], writes=[ys_], out=ys_.t[:, 512:1024], in_=py.t[:])
            DMA(act, d_ys[j % NW], YS_d[j * 128:(j + 1) * 128, :], ys_.t[:], reads=[ys_], writes=[B_YS])
        m.barrier()

    with ExitStack() as es:
        NF = 4
        y0 = [tl(f"y0{i}", [128, D], F32, es) for i in range(NF)]
        y1 = [tl(f"y1{i}", [128, D], F32, es) for i in range(NF)]
        xf = [tl(f"xf{i}", [128, D], F32, es) for i in range(NF)]
        acc = [tl(f"acc{i}", [128, D], F32, es) for i in range(NF)]
        t32b = tl("t32b", [128, 32], F32, es)
        adjs = tl("adjs", [128, NT, 2], F32, es)
        yidx = tl("yidx", [128, NT, 2], I32, es)
        d_g0 = [m.dsem(f"d_g0{i}") for i in range(NF)]
        d_g1 = [m.dsem(f"d_g1{i}") for i in range(NF)]
        d_xf = [m.dsem(f"d_xf{i}") for i in range(NF)]
        d_of = [m.dsem(f"d_of{i}") for i in range(NF)]
        for t in range(NT):
            for kk in range(2):
                O(dve, "tensor_tensor", reads=[OH, ADJ], writes=[t32b], out=t32b.t[:], in0=OH.t[:, t, kk, :], in1=ADJ.t[:], op=ALU.mult)
                O(dve, "tensor_reduce", reads=[t32b], writes=[adjs], out=adjs.t[:, t, kk:kk + 1], in_=t32b.t[:], axis=AX.X, op=ALU.add)
        O(dve, "tensor_tensor", reads=[PF, adjs], writes=[adjs], out=adjs.t[:], in0=PF.t[:], in1=adjs.t[:], op=ALU.subtract)
        O(dve, "tensor_scalar", reads=[adjs], writes=[adjs], out=adjs.t[:], in0=adjs.t[:], scalar1=float(NTILE * 128 - 1), scalar2=0.0,
          op0=ALU.min, op1=ALU.max)
        O(dve, "tensor_copy", reads=[adjs], writes=[yidx], out=yidx.t[:], in_=adjs.t[:])

        def load_fin(t):
            i = t % NF
            m.dma(pool, d_g0[i], lambda: pool.h.indirect_dma_start(
                out=y0[i].t[:], out_offset=None, in_=YS_d, in_offset=bass.IndirectOffsetOnAxis(ap=yidx.t[:, t, 0:1], axis=0)),
                reads=[B_YS, yidx.b], writes=[y0[i].b])
            m.dma(pool, d_g1[i], lambda: pool.h.indirect_dma_start(
                out=y1[i].t[:], out_offset=None, in_=YS_d, in_offset=bass.IndirectOffsetOnAxis(ap=yidx.t[:, t, 1:2], axis=0)),
                reads=[B_YS, yidx.b], writes=[y1[i].b])
            DMA(act, d_xf[i], xf[i].t[:], out_d[t * 128:(t + 1) * 128, :], reads=[B_OUT], writes=[xf[i]])

        for t in range(NF - 1):
            load_fin(t)
        B_OUT2 = Buf()
        for t in range(NT):
            if t + NF - 1 < NT:
                load_fin(t + NF - 1)
            i = t % NF
            a = acc[i]
            O(act, "activation", reads=[y0[i], Wt], writes=[a], out=a.t[:], in_=y0[i].t[:], func=AF.Identity, scale=Wt.t[:, t, 0:1])
            O(dve, "scalar_tensor_tensor", reads=[y1[i], Wt, a], writes=[a], out=a.t[:], in0=y1[i].t[:], scalar=Wt.t[:, t, 1:2],
              in1=a.t[:], op0=ALU.mult, op1=ALU.add)
            O(dve, "tensor_tensor", reads=[a, modB], writes=[a], out=a.t[:], in0=a.t[:], in1=GT_F, op=ALU.mult)
            O(dve if t % 2 == 0 else pool, "tensor_tensor", reads=[a, xf[i]], writes=[a], out=a.t[:], in0=a.t[:], in1=xf[i].t[:], op=ALU.add)
            DMA(sp, d_of[i], out_d[t * 128:(t + 1) * 128, :], a.t[:], reads=[a, xf[i]], writes=[B_OUT2])
        m.barrier()
    m.es.close()
    return nc


def prep_inputs(inputs):
    f = lambda a: np.ascontiguousarray(np.asarray(a))
    g = lambda name: np.asarray(inputs[name])[0]
    shared = {
        "w_ada": f(g("w_ada")),
        "b_ada": f(g("b_ada").reshape(1, -1)),
        "norm_mix": f(g("norm_mix").reshape(1, -1)),
        "w_in": f(g("w_in")),
        "conv_w_c": f(g("conv_w").reshape(31, 4, 128).transpose(2, 1, 0)),
        "conv_b_c": f(g("conv_b").reshape(4, 128).T),
        "conv_ln_g_c": f(g("conv_ln_g").reshape(4, 128).T),
        "conv_ln_b_c": f(g("conv_ln_b").reshape(4, 128).T),
        "q_a_norm_c": f(g("q_a_norm").reshape(2, 128).T),
        "w_q_b": f(g("w_q_b")),
        "kv_a_norm_c": f(g("kv_a_norm").reshape(1, 128).T),
        "w_kv_b": f(g("w_kv_b")),
        "q_norm": f(g("q_norm").reshape(1, -1)),
        "k_norm": f(g("k_norm").reshape(1, -1)),
        "w_out": f(g("w_out")),
        "norm_ffn": f(g("norm_ffn").reshape(1, -1)),
        "w_router": f(np.concatenate([g("w_group"), g("w_expert")], axis=1)),
        "b_router": f(np.concatenate([g("b_group"), g("b_expert")]).reshape(1, -1)),
        "w_gate_e": f(g("w_gate_e")),
        "w_up_e": f(g("w_up_e")),
        "w_down_e": f(g("w_down_e")),
    }
    x = np.asarray(inputs["x"])
    c = np.asarray(inputs["c"])
    pos = np.asarray(inputs["positions"])
    maps = []
    for b in range(8):
        d = dict(shared)
        d["x"] = f(x[b])
        d["c_pk"] = f(c[b].reshape(8, 128).T)
        d["pos_pj"] = f(pos[b].reshape(NT, 128).T.astype(np.int32))
        maps.append(d)
    return maps


_NC = None


def kernel(**inputs):
    global _NC
    if _NC is None:
        _NC = build()
    maps = prep_inputs(inputs)
    res = run_bass_kernel_spmd(_NC, maps, core_ids=list(range(8)))
    return np.stack([np.asarray(r["out"]) for r in res.results], axis=0).astype(np.float32)
```
